# Optimizing a Trainium2 kernel written in Bass

```python
import math
import jax, jax.numpy as jnp
from jax import lax
import numpy as np

D_MODEL = 1024
BATCH = 4
SEQ = 4096
DEPTH = 2

N_MEM = 256
CHUNK = 128
A_GROUPS = 8
A_WIDTH = 1024
A_GROUP_DIM = A_WIDTH // A_GROUPS
B_HEADS = 10
B_WIDTH = 1280
B_HEAD_DIM = B_WIDTH // B_HEADS
CONV_WIDTH = 4
LRU_C = 8.0
C_HEADS = 4
C_HEAD_DIM = 256
C_WIDTH = C_HEADS * C_HEAD_DIM
N_BRANCH = 3
IN_WIDTH = 2 * A_WIDTH + 2 * B_WIDTH + C_WIDTH + N_BRANCH * D_MODEL
SPLIT_POINTS = (A_WIDTH, 2 * A_WIDTH, 2 * A_WIDTH + B_WIDTH, 2 * A_WIDTH + 2 * B_WIDTH,
                2 * A_WIDTH + 2 * B_WIDTH + C_WIDTH)
N_GROUPS = 4
EXPERTS_PER_GROUP = 4
N_EXPERTS = N_GROUPS * EXPERTS_PER_GROUP
TOP_K = 2
D_EXPERT = 256
ALPHA = (2 * DEPTH) ** 0.25
BETA = (8 * DEPTH) ** -0.25
LN_EPS = 1e-5

kernel_name = "hybrid_gmlp_rglru_memattn_hmoe_deepnorm"


def layer_norm(x, g, b):
    xf = x.astype(jnp.float32)
    mu = jnp.mean(xf, axis=-1, keepdims=True)
    var = jnp.mean(jnp.square(xf - mu), axis=-1, keepdims=True)
    y = (xf - mu) * lax.rsqrt(var + LN_EPS)
    return (y * g.astype(jnp.float32) + b.astype(jnp.float32)).astype(x.dtype)


def spatial_gating(u, v, ln_g, ln_b, w_s, b_s):
    bsz, seq, _ = v.shape
    n_chunks = seq // CHUNK
    v = layer_norm(v, ln_g, ln_b)
    causal = jnp.tril(jnp.ones((CHUNK, CHUNK), dtype=bool))
    w = jnp.where(causal[None], w_s, jnp.zeros_like(w_s))
    vc = v.reshape(bsz, n_chunks, CHUNK, A_GROUPS, A_GROUP_DIM)
    s = jnp.einsum('gts,bcsgd->bctgd', w, vc) + b_s.T[None, None, :, :, None]
    return u * s.reshape(bsz, seq, A_WIDTH)


def causal_depthwise_conv(x, w, b):
    seq = x.shape[1]
    xp = jnp.pad(x, ((0, 0), (CONV_WIDTH - 1, 0), (0, 0)))
    out = b
    for k in range(CONV_WIDTH):
        out = out + w[k] * xp[:, k:k + seq]
    return out


def rg_lru(x, w_a, b_a, w_x, b_x, lam):
    bsz, seq, _ = x.shape
    xh = x.reshape(bsz, seq, B_HEADS, B_HEAD_DIM)
    r = jax.nn.sigmoid(jnp.einsum('bshi,hij->bshj', xh, w_a).reshape(bsz, seq, B_WIDTH) + b_a)
    i = jax.nn.sigmoid(jnp.einsum('bshi,hij->bshj', xh, w_x).reshape(bsz, seq, B_WIDTH) + b_x)
    log_a = -LRU_C * r.astype(jnp.float32) * jax.nn.softplus(-lam.astype(jnp.float32))
    a = jnp.exp(log_a)
    mult = jnp.sqrt(-jnp.expm1(2.0 * log_a))
    bx = mult * (i * x).astype(jnp.float32)

    def combine(c1, c2):
        a1, b1 = c1
        a2, b2 = c2
        return a1 * a2, a2 * b1 + b2

    _, h = lax.associative_scan(combine, (a, bx), axis=1)
    return h.astype(x.dtype)


def memory_attention(q, mem_n, w_kv):
    bsz, seq, _ = q.shape
    kv = jnp.einsum('bmd,de->bme', mem_n, w_kv)
    k, v = jnp.split(kv, 2, axis=-1)
    qh = q.reshape(bsz, seq, C_HEADS, C_HEAD_DIM)
    kh = k.reshape(bsz, N_MEM, C_HEADS, C_HEAD_DIM)
    vh = v.reshape(bsz, N_MEM, C_HEADS, C_HEAD_DIM)
    scores = jnp.einsum('bshd,bmhd->bhsm', qh, kh).astype(jnp.float32) * (C_HEAD_DIM ** -0.5)
    p = jax.nn.softmax(scores, axis=-1).astype(q.dtype)
    o = jnp.einsum('bhsm,bmhd->bshd', p, vh)
    return o.reshape(bsz, seq, C_WIDTH)


def hybrid_mixer(x, mem_n, w_in, b_in, ln_v_g, ln_v_b, w_s, b_s, conv_w, conv_b,
                 w_a, b_a, w_x, b_x, lam, w_kv, p_a, p_b, p_c, w_o, b_o):
    z = jnp.einsum('bsd,de->bse', x, w_in) + b_in
    u_a, v_a, x_b, gate_b, q_c, merge = jnp.split(z, SPLIT_POINTS, axis=-1)
    o_a = spatial_gating(jax.nn.gelu(u_a), jax.nn.gelu(v_a), ln_v_g, ln_v_b, w_s, b_s)
    o_b = rg_lru(causal_depthwise_conv(x_b, conv_w, conv_b), w_a, b_a, w_x, b_x, lam) * jax.nn.gelu(gate_b)
    o_c = memory_attention(q_c, mem_n, w_kv)
    g_a, g_b, g_c = jnp.split(jax.nn.sigmoid(merge), N_BRANCH, axis=-1)
    y = (g_a * jnp.einsum('bse,ed->bsd', o_a, p_a)
         + g_b * jnp.einsum('bse,ed->bsd', o_b, p_b)
         + g_c * jnp.einsum('bse,ed->bsd', o_c, p_c))
    return jnp.einsum('bsd,de->bse', y, w_o) + b_o


def hierarchical_moe(x, w_rg, b_rg, w_re, b_re, w_up, w_down):
    bsz, seq, d = x.shape
    xf = x.reshape(bsz * seq, d)
    n_tok = xf.shape[0]
    group_p = jax.nn.softmax((xf @ w_rg + b_rg).astype(jnp.float32), axis=-1)
    p_top, g_top = lax.top_k(group_p, 1)
    exp_logits = (xf @ w_re + b_re).astype(jnp.float32).reshape(n_tok, N_GROUPS, EXPERTS_PER_GROUP)
    in_group = exp_logits[jnp.arange(n_tok), g_top[:, 0]]
    e_p, e_idx = lax.top_k(jax.nn.softmax(in_group, axis=-1), TOP_K)
    weights = p_top * e_p / jnp.sum(e_p, axis=-1, keepdims=True)
    expert_id = g_top * EXPERTS_PER_GROUP + e_idx
    gate = jnp.sum(jax.nn.one_hot(expert_id, N_EXPERTS, dtype=jnp.float32) * weights[..., None],
                   axis=1).astype(x.dtype)
    h = jnp.einsum('nd,edf->nef', xf, w_up)
    h_g, h_u = jnp.split(h, 2, axis=-1)
    h = jax.nn.silu(h_g) * h_u * gate[:, :, None]
    y = jnp.einsum('nef,efd->nd', h, w_down)
    return y.reshape(bsz, seq, d)


def setup_inputs(seed: int = 0) -> dict:
    key = jax.random.key(seed)
    ks = jax.random.split(key, 40)
    f32 = jnp.float32

    def nrm(k, shape, scale):
        return jax.random.normal(k, shape, dtype=f32) * scale

    lam_u = jax.random.uniform(ks[18], (DEPTH, B_WIDTH), dtype=f32, minval=0.9, maxval=0.999)
    a0 = lam_u ** (1.0 / LRU_C)
    lam = jnp.log(a0) - jnp.log1p(-a0)
    return {
        "x": nrm(ks[0], (BATCH, SEQ, D_MODEL), 1.0),
        "mem": nrm(ks[1], (BATCH, N_MEM, D_MODEL), 1.0),
        "ln_in_g": 1.0 + nrm(ks[2], (D_MODEL,), 0.02),
        "ln_in_b": nrm(ks[3], (D_MODEL,), 0.02),
        "ln_mem_g": 1.0 + nrm(ks[4], (D_MODEL,), 0.02),
        "ln_mem_b": nrm(ks[5], (D_MODEL,), 0.02),
        "w_in": nrm(ks[6], (DEPTH, D_MODEL, IN_WIDTH), D_MODEL ** -0.5),
        "b_in": nrm(ks[7], (DEPTH, IN_WIDTH), 0.02),
        "ln_v_g": 1.0 + nrm(ks[8], (DEPTH, A_WIDTH), 0.02),
        "ln_v_b": nrm(ks[9], (DEPTH, A_WIDTH), 0.02),
        "w_s": nrm(ks[10], (DEPTH, A_GROUPS, CHUNK, CHUNK), CHUNK ** -0.5),
        "b_s": 1.0 + nrm(ks[11], (DEPTH, A_GROUPS, CHUNK), 0.02),
        "conv_w": nrm(ks[12], (DEPTH, CONV_WIDTH, B_WIDTH), CONV_WIDTH ** -0.5),
        "conv_b": nrm(ks[13], (DEPTH, B_WIDTH), 0.02),
        "w_a": nrm(ks[14], (DEPTH, B_HEADS, B_HEAD_DIM, B_HEAD_DIM), B_HEAD_DIM ** -0.5),
        "b_a": nrm(ks[15], (DEPTH, B_WIDTH), 0.02),
        "w_x": nrm(ks[16], (DEPTH, B_HEADS, B_HEAD_DIM, B_HEAD_DIM), B_HEAD_DIM ** -0.5),
        "b_x": nrm(ks[17], (DEPTH, B_WIDTH), 0.02),
        "lam": lam,
        "w_kv": nrm(ks[19], (DEPTH, D_MODEL, 2 * C_WIDTH), D_MODEL ** -0.5),
        "p_a": nrm(ks[20], (DEPTH, A_WIDTH, D_MODEL), A_WIDTH ** -0.5),
        "p_b": nrm(ks[21], (DEPTH, B_WIDTH, D_MODEL), B_WIDTH ** -0.5),
        "p_c": nrm(ks[22], (DEPTH, C_WIDTH, D_MODEL), C_WIDTH ** -0.5),
        "w_o": nrm(ks[23], (DEPTH, D_MODEL, D_MODEL), BETA * D_MODEL ** -0.5),
        "b_o": nrm(ks[24], (DEPTH, D_MODEL), 0.02),
        "ln1_g": 1.0 + nrm(ks[25], (DEPTH, D_MODEL), 0.02),
        "ln1_b": nrm(ks[26], (DEPTH, D_MODEL), 0.02),
        "w_rg": nrm(ks[27], (DEPTH, D_MODEL, N_GROUPS), D_MODEL ** -0.5),
        "b_rg": nrm(ks[28], (DEPTH, N_GROUPS), 0.01),
        "w_re": nrm(ks[29], (DEPTH, D_MODEL, N_EXPERTS), D_MODEL ** -0.5),
        "b_re": nrm(ks[30], (DEPTH, N_EXPERTS), 0.01),
        "w_up": nrm(ks[31], (DEPTH, N_EXPERTS, D_MODEL, 2 * D_EXPERT), D_MODEL ** -0.5),
        "w_down": nrm(ks[32], (DEPTH, N_EXPERTS, D_EXPERT, D_MODEL), BETA * D_EXPERT ** -0.5),
        "ln2_g": 1.0 + nrm(ks[33], (DEPTH, D_MODEL), 0.02),
        "ln2_b": nrm(ks[34], (DEPTH, D_MODEL), 0.02),
    }


def reference(x, mem, ln_in_g, ln_in_b, ln_mem_g, ln_mem_b, w_in, b_in, ln_v_g, ln_v_b,
              w_s, b_s, conv_w, conv_b, w_a, b_a, w_x, b_x, lam, w_kv, p_a, p_b, p_c,
              w_o, b_o, ln1_g, ln1_b, w_rg, b_rg, w_re, b_re, w_up, w_down, ln2_g, ln2_b):
    x = layer_norm(x, ln_in_g, ln_in_b)
    mem_n = layer_norm(mem, ln_mem_g, ln_mem_b)
    for l in range(DEPTH):
        m = hybrid_mixer(x, mem_n, w_in[l], b_in[l], ln_v_g[l], ln_v_b[l], w_s[l], b_s[l],
                         conv_w[l], conv_b[l], w_a[l], b_a[l], w_x[l], b_x[l], lam[l],
                         w_kv[l], p_a[l], p_b[l], p_c[l], w_o[l], b_o[l])
        x = layer_norm(ALPHA * x + m, ln1_g[l], ln1_b[l])
        f = hierarchical_moe(x, w_rg[l], b_rg[l], w_re[l], b_re[l], w_up[l], w_down[l])
        x = layer_norm(ALPHA * x + f, ln2_g[l], ln2_b[l])
    return x
```

```python
import numpy as np
import concourse.bass as bass
import concourse.mybir as mybir
from concourse.bass_utils import run_bass_kernel_spmd

F32 = mybir.dt.float32
BF16 = mybir.dt.bfloat16
AF = mybir.ActivationFunctionType
ALU = mybir.AluOpType
AX = mybir.AxisListType

ENGS = ("pe", "act", "dve", "pool", "sp")

DEPTH = 2
NCORES = 8
NT = 2048
TB = 512
NB = NT // TB
ALPHA = float((2 * DEPTH) ** 0.25)
LN_EPS = 1e-5
NEG = -1.0e30
NO_SAME_ENGINE_SYNC = ("pe", "sp")


class Sched:
    def __init__(self, nc):
        self.nc = nc
        self.ops = {e: [] for e in ENGS}
        self.cnt = {e: 0 for e in ENGS}
        self.sem = {}
        self.dma_cnt = {}
        self.last_w = {}
        self.readers = {}
        self.seen = {e: {} for e in ENGS}
        self._ctx = []

    def _get_sem(self, name):
        if name not in self.sem:
            cm = self.nc.semaphore(name)
            s = cm.__enter__()
            self._ctx.append(cm)
            self.sem[name] = s
        return self.sem[name]

    def _waits_for(self, eng, reads, writes, same_engine_raw=True):
        need = {}

        def add(ev):
            if ev is None:
                return
            sname, val, weng = ev
            if weng == eng and (not same_engine_raw or eng in NO_SAME_ENGINE_SYNC):
                return
            if need.get(sname, 0) < val:
                need[sname] = val

        for k in reads:
            add(self.last_w.get(k))
        for k in writes:
            add(self.last_w.get(k))
            for ev in self.readers.get(k, []):
                if ev[2] == eng:
                    continue
                add(ev)
        out = []
        for sname, val in need.items():
            if self.seen[eng].get(sname, 0) >= val:
                continue
            self.seen[eng][sname] = val
            out.append((sname, val))
        return out

    def _record(self, ev, reads, writes):
        for k in reads:
            self.readers.setdefault(k, []).append(ev)
        for k in writes:
            self.last_w[k] = ev
            self.readers[k] = []

    def op(self, eng, fn, reads=(), writes=()):
        waits = self._waits_for(eng, reads, writes)
        self.cnt[eng] += 1
        ev = ("prog_" + eng, self.cnt[eng], eng)
        self._get_sem(ev[0])
        for s, _ in waits:
            self._get_sem(s)
        self.ops[eng].append(("op", fn, waits, ev))
        self._record(ev, reads, writes)
        return ev

    def dma(self, eng, fn, key, n=1, reads=(), writes=()):
        waits = self._waits_for(eng, reads, writes, same_engine_raw=True)
        sname = "dma_" + str(key)
        self._get_sem(sname)
        self.dma_cnt[sname] = self.dma_cnt.get(sname, 0) + 16 * n
        ev = (sname, self.dma_cnt[sname], "dma:" + str(key))
        for s, _ in waits:
            self._get_sem(s)
        self.ops[eng].append(("dma", fn, waits, ev))
        self._record(ev, reads, writes)
        return ev

    def wait_all(self, eng, keys):
        waits = self._waits_for(eng, keys, (), same_engine_raw=False)
        self.ops[eng].append(("wait", None, waits, None))

    def emit(self):
        nc = self.nc
        sem = self.sem
        ops = self.ops

        def replay(e, name):
            for kind, fn, waits, ev in ops[name]:
                for s, v in waits:
                    e.wait_ge(sem[s], v)
                if kind == "op":
                    ins = fn(e)
                    ins.then_inc(sem[ev[0]], 1)
                elif kind == "dma":
                    s = sem[ev[0]]
                    fn(e, lambda ins, s=s: ins.then_inc(s, 16))

        with nc.Block() as block:
            @block.tensor
            def _(e):
                replay(e, "pe")

            @block.scalar
            def _(e):
                replay(e, "act")

            @block.vector
            def _(e):
                replay(e, "dve")

            @block.gpsimd
            def _(e):
                replay(e, "pool")

            @block.sync
            def _(e):
                replay(e, "sp")

    def close(self):
        for cm in reversed(self._ctx):
            cm.__exit__(None, None, None)


def build_program(n_layers=DEPTH, dbg=None):
    nc = bass.Bass("TRN2", target_bir_lowering=False)
    S = Sched(nc)

    def din(name, shape, dt=F32):
        return nc.dram_tensor(name, list(shape), dt, kind="ExternalInput").ap()

    xT_d = din("xT", [1024, NT])
    memT_d = din("memT", [1024, 256])
    flag_d = din("flag", [128, 1])
    ln0_d = din("ln0", [128, 8, 4])
    w_in_d = din("w_in", [DEPTH, 1024, 8704])
    binfm_d = din("bin_fm", [DEPTH, 128, 68])
    brow_d = din("brow", [DEPTH, 3, 1024])
    lnv_d = din("lnv", [DEPTH, 2, 1024])
    wsT_d = din("wsT", [DEPTH, 128, 8, 128])
    bpar_d = din("bpar", [DEPTH, 128, 10, 8])
    wax_d = din("wax", [DEPTH, 2, 10, 128, 128])
    w_kv_d = din("w_kv", [DEPTH, 1024, 2048])
    p_a_d = din("p_a", [DEPTH, 1024, 1024])
    p_b_d = din("p_b", [DEPTH, 1280, 1024])
    p_c_d = din("p_c", [DEPTH, 1024, 1024])
    w_o_d = din("w_o", [DEPTH, 1024, 1024])
    lnp_d = din("lnp", [DEPTH, 128, 8, 4])
    w_r_d = din("w_r", [DEPTH, 128, 8, 20])
    b_r_d = din("b_r", [DEPTH, 1, 20])
    w_up_d = din("w_up", [DEPTH, 16, 1024, 512])
    w_dn_d = din("w_dn", [DEPTH, 8, 128, 32 * 128])
    out_d = nc.dram_tensor("out", [1024, NT], F32, kind="ExternalOutput").ap()
    cc_in = nc.dram_tensor("cc_in", [128, 40], F32).ap()
    cc_out = nc.dram_tensor("cc_out", [256, 40], F32).ap()

    def sb(name, shape, dt):
        return nc.alloc_sbuf_tensor("s_" + name, list(shape), dt)

    x32 = sb("x32", [128, 8, NT], F32)
    xb = sb("xb", [128, 8, TB], BF16)
    NPG = 25
    arena = sb("arena", [128, NPG * 512], F32)
    arena_bf = arena[:].bitcast(BF16)

    def PG(i):
        return arena[:, i * 512:(i + 1) * 512], ("pg", i)

    def HP(j):
        return arena_bf[:, j * 512:(j + 1) * 512], ("pg", j // 2)

    gv = sb("gv", [128, 1024], F32)
    vn = sb("vn", [128, 1024], BF16)
    XR = sb("XR", [128, 516], F32)
    NSLOT = 4
    wt = [sb("wt%d" % i, [128, 8 * 512], BF16) for i in range(NSLOT)]
    KT = sb("KT", [128, 8, 256], BF16)
    Vt = sb("Vt", [128, 2, 1024], BF16)
    memT = sb("memTb", [128, 8, 256], BF16)
    lnvbc = sb("lnvbc", [128, 2, 1024], F32)
    WmT = sb("WmT", [128, 8, 128], BF16)
    waxb = sb("waxb", [128, 2, 10, 128], BF16)
    brow = sb("browb", [33, 3072], BF16)
    w_r = sb("w_r", [128, 8, 20], F32)
    b_r = sb("b_r", [1, 20], F32)
    bpar = sb("bpar", [128, 10, 8], F32)
    negc = sb("negc", [128, 10], F32)
    neg2c = sb("neg2c", [128, 10], F32)
    sptmp = sb("sptmp", [128, 10], F32)
    lnp = sb("lnp", [128, 8, 4], F32)
    ln0 = sb("ln0", [128, 8, 4], F32)
    binfm = sb("binfm", [128, 68], F32)
    identb = sb("identb", [128, 128], BF16)
    onesS = sb("onesS", [128, 128], BF16)
    onesB = sb("onesB", [128, 128], BF16)
    ones33 = sb("ones33", [33, 512], BF16)
    ones32r = sb("ones32r", [1, 128], F32)
    SEL2 = sb("SEL2", [32, 16, 128], BF16)
    flag = sb("flag", [128, 1], F32)
    HC = sb("HC", [128, 10], F32)
    HALO = sb("HALO", [128, 10, 3], F32)
    EX = sb("EX", [128, 40], F32)
    RX = sb("RX", [128, 40], F32)
    stat = sb("stat", [128, 16], F32)
    rt = sb("rt", [128, 4, 96], F32)
    G2 = sb("G2", [128, 4, 32], BF16)
    zero512 = None

    psb = [nc.alloc_psum_tensor("ps%d" % i, [128, 512], F32) for i in range(8)]
    ps_ctr = [0]

    ps_pinned = set()

    def nextps(pin=False):
        while True:
            i = ps_ctr[0] % 8
            ps_ctr[0] += 1
            if i not in ps_pinned:
                break
        if pin:
            ps_pinned.add(i)
        return psb[i], ("ps", i)

    def unpin(key):
        ps_pinned.discard(key[1])

    def PE(fn, r, w):
        return S.op("pe", fn, reads=r, writes=w)

    def ACT(fn, r, w):
        return S.op("act", fn, reads=r, writes=w)

    def DVE(fn, r, w):
        return S.op("dve", fn, reads=r, writes=w)

    def POOL(fn, r, w):
        return S.op("pool", fn, reads=r, writes=w)

    def LOAD(dst, src, key, q="sp"):
        S.dma(q, lambda e, inc: inc(e.dma_start(out=dst, in_=src)), key, writes=[key])

    w_ctr = [0]

    def wload(src_view, K, C):
        s = w_ctr[0] % NSLOT
        w_ctr[0] += 1
        dst = wt[s][:, 0:K * C].rearrange("p (k c) -> p k c", k=K)
        key = ("w", s)
        S.dma("pool", lambda e, inc: inc(e.dma_start(out=dst, in_=src_view)), "w%d" % s, writes=[key])
        return dst, key

    def win_view(l, c0, C):
        return w_in_d[l, :, c0:c0 + C].rearrange("(k p) c -> p k c", p=128)

    def mat_view(d, l, r0, K, c0, C):
        return d[l, r0:r0 + K * 128, c0:c0 + C].rearrange("(k p) c -> p k c", p=128)

    POOL(lambda e: e.memset(onesS[:], 1.0 / 1024.0), [], ["onesS"])
    POOL(lambda e: e.memset(onesB[:], 1.0), [], ["onesB"])
    POOL(lambda e: e.memset(ones33[:], 1.0), [], ["ones33"])
    POOL(lambda e: e.memset(ones32r[:], 1.0), [], ["ones32r"])
    onesrc = arena[:, 12 * 512:13 * 512]
    POOL(lambda e: e.memset(arena[:, 12 * 512:16 * 512], 1.0), [], [("pg", 12), ("pg", 13), ("pg", 14), ("pg", 15)])
    selA = arena[0:32, 0:2048].rearrange("p (a m) -> p a m", a=16)
    selB = arena[0:32, 2048:4096].rearrange("p (a m) -> p a m", a=16)
    onesel = arena[0:32, 12 * 512:16 * 512].rearrange("p (a m) -> p a m", a=16)
    POOL(lambda e: e.affine_select(out=selA, in_=onesel, pattern=[[-1, 16], [0, 128]],
                                   compare_op=ALU.is_equal, fill=0.0, base=0, channel_multiplier=1),
         [("pg", i) for i in range(12, 16)], [("pg", i) for i in range(0, 4)])
    POOL(lambda e: e.affine_select(out=selB, in_=onesel, pattern=[[-1, 16], [0, 128]],
                                   compare_op=ALU.is_equal, fill=0.0, base=-16, channel_multiplier=1),
         [("pg", i) for i in range(12, 16)], [("pg", i) for i in range(4, 8)])
    POOL(lambda e: e.tensor_tensor(out=SEL2[:], in0=selA, in1=selB, op=ALU.add),
         [("pg", i) for i in range(0, 8)], ["SEL2"])
    POOL(lambda e: e.affine_select(out=identb[:], in_=onesrc[:, 0:128], pattern=[[-1, 128]], compare_op=ALU.is_equal,
                                   fill=0.0, base=0, channel_multiplier=1), [("pg", 12)], ["identb"])
    LOAD(flag[:], flag_d, "flag")
    LOAD(ln0[:], ln0_d, "ln0")

    def ln_fm(src, dst32, dstbf, gcol, bcol, N, tagk):
        psm, kpm = nextps()
        pse, kpe = nextps()
        rbk = []
        for k in range(8):
            s_ap, s_key = src(k)
            rb_ap, rb_key = HP(16 + k)
            sq_ap, sq_key = HP(24 + k)
            ACT(lambda e, o=rb_ap, i=s_ap: e.activation(out=o[:, 0:N], in_=i, func=AF.Copy), [s_key], [rb_key])
            ACT(lambda e, o=sq_ap, i=s_ap: e.activation(out=o[:, 0:N], in_=i, func=AF.Square), [s_key], [sq_key])
            rbk.append((rb_ap, rb_key, sq_ap, sq_key))

        def mm_stats(e, which):
            last = None
            for k in range(8):
                ap = rbk[k][0] if which == 0 else rbk[k][2]
                dst = psm if which == 0 else pse
                last = e.matmul(dst[:, 0:N], lhsT=onesS[:], rhs=ap[:, 0:N], start=(k == 0), stop=(k == 7))
            return last
        PE(lambda e: mm_stats(e, 0), [r[1] for r in rbk] + ["onesS"], [kpm])
        PE(lambda e: mm_stats(e, 1), [r[3] for r in rbk] + ["onesS"], [kpe])
        mean, kmean = PG(17)
        msq, kmsq = PG(18)
        var, kvar = PG(19)
        rstd_t, krstd = nextps()
        nmr_t, knmr = nextps()
        rstd = rstd_t[:]
        nmr = nmr_t[:]
        ACT(lambda e: e.activation(out=mean[:, 0:N], in_=psm[:, 0:N], func=AF.Copy), [kpm], [kmean])
        ACT(lambda e: e.activation(out=msq[:, 0:N], in_=psm[:, 0:N], func=AF.Square), [kpm], [kmsq])
        DVE(lambda e: e.tensor_tensor(out=var[:, 0:N], in0=pse[:, 0:N], in1=msq[:, 0:N], op=ALU.subtract),
            [kpe, kmsq], [kvar])
        DVE(lambda e: e.tensor_scalar(out=var[:, 0:N], in0=var[:, 0:N], scalar1=0.0, scalar2=LN_EPS,
                                      op0=ALU.max, op1=ALU.add), [kvar], [kvar])
        ACT(lambda e: e.activation(out=var[:, 0:N], in_=var[:, 0:N], func=AF.Sqrt), [kvar], [kvar])
        DVE(lambda e: e.reciprocal(out=rstd[:, 0:N], in_=var[:, 0:N]), [kvar], [krstd])
        DVE(lambda e: e.scalar_tensor_tensor(out=nmr[:, 0:N], in0=mean[:, 0:N], scalar=-1.0, in1=rstd[:, 0:N],
                                             op0=ALU.mult, op1=ALU.mult), [kmean, krstd], [knmr])
        for k in range(8):
            s_ap, s_key = src(k)
            ta, kta = PG(22 + (k % 2))
            DVE(lambda e, o=ta, i=s_ap: e.tensor_tensor(out=o[:, 0:N], in0=rstd[:, 0:N], in1=i, op=ALU.mult),
                [s_key, krstd], [kta])
            DVE(lambda e, o=ta: e.tensor_tensor(out=o[:, 0:N], in0=nmr[:, 0:N], in1=o[:, 0:N], op=ALU.add),
                [kta, knmr], [kta])
            if dst32 is not None:
                d_ap, d_key = dst32(k)
                ACT(lambda e, o=d_ap, i=ta, k=k: e.activation(out=o, in_=i[:, 0:N], func=AF.Identity,
                                                               scale=gcol(k), bias=bcol(k)),
                    [kta, tagk], [d_key])
                if dstbf is not None:
                    b_ap, b_key = dstbf(k)
                    ACT(lambda e, o=b_ap, i=d_ap: e.activation(out=o, in_=i, func=AF.Copy), [d_key], [b_key])
            else:
                b_ap, b_key = dstbf(k)
                ACT(lambda e, o=b_ap, i=ta, k=k: e.activation(out=o, in_=i[:, 0:N], func=AF.Identity,
                                                               scale=gcol(k), bias=bcol(k)),
                    [kta, tagk], [b_key])

    def xs(k, blk):
        return x32[:, k, blk * TB:(blk + 1) * TB], ("x32", k, blk)

    def xbk(k):
        return xb[:, k, :], ("xb", k)

    for blk in range(NB):
        S.dma("sp", lambda e, inc, blk=blk: inc(e.dma_start(
            out=x32[:, :, blk * TB:(blk + 1) * TB],
            in_=xT_d[:, blk * TB:(blk + 1) * TB].rearrange("(k p) t -> p k t", p=128))),
            "xin%d" % blk, writes=[("x32", k, blk) for k in range(8)])
    mst = arena[:, 0:2048].rearrange("p (k t) -> p k t", k=8)
    S.dma("sp", lambda e, inc: inc(e.dma_start(out=mst, in_=memT_d.rearrange("(k p) t -> p k t", p=128))),
          "memin", writes=[("pg", i) for i in range(4)])
    ln_fm(lambda k: (mst[:, k, :], ("pg", k // 2)), None, lambda k: (memT[:, k, :], ("memT", k)),
          lambda k: ln0[:, k, 2:3], lambda k: ln0[:, k, 3:4], 256, "ln0")
    for blk in range(NB):
        ln_fm(lambda k, blk=blk: xs(k, blk), lambda k, blk=blk: xs(k, blk), None,
              lambda k: ln0[:, k, 0:1], lambda k: ln0[:, k, 1:2], TB, "ln0")

    def load_layer_consts(l):
        LOAD(binfm[:], binfm_d[l], "binfm")
        LOAD(lnp[:], lnp_d[l], "lnp")
        LOAD(bpar[:], bpar_d[l], "bpar")
        LOAD(w_r[:], w_r_d[l], "w_r")
        LOAD(b_r[:], b_r_d[l], "b_r")
        LOAD(lnvbc[:, 0, :], lnv_d[l, 0].partition_broadcast(128), "lnvg")
        LOAD(lnvbc[:, 1, :], lnv_d[l, 1].partition_broadcast(128), "lnvb")
        S.dma("pool", lambda e, inc: inc(e.dma_start(out=WmT[:], in_=wsT_d[l])), "WmT", writes=["WmT"])
        POOL(lambda e: e.affine_select(out=WmT[:], in_=WmT[:], pattern=[[0, 8], [1, 128]], compare_op=ALU.is_ge,
                                       fill=0.0, base=0, channel_multiplier=-1), ["WmT"], ["WmT"])
        S.dma("pool", lambda e, inc: inc(e.dma_start(out=waxb[:], in_=wax_d[l].rearrange("a h i j -> i a h j"))),
              "waxb", writes=["waxb"])
        POOL(lambda e: e.memset(brow[:], 0.0), [], ["brow"])
        st, kst0 = PG(22)
        st2, kst1 = PG(23)
        stg = arena[:, 22 * 512:24 * 512]
        browhi32 = arena_bf[:, 42 * 512:44 * 512]
        for j in range(3):
            S.dma("pool", lambda e, inc, j=j: inc(e.dma_start(out=brow[0:1, j * 1024:(j + 1) * 1024],
                                                              in_=brow_d[l, j:j + 1, :])),
                  "browhi%d" % j, reads=["brow"], writes=["brow"])
            S.dma("pool", lambda e, inc, j=j: inc(e.dma_start(out=browhi32[32:33, :], in_=brow_d[l, j:j + 1, :])),
                  "browhi32", writes=[("pg", 21)])
            S.dma("sp", lambda e, inc, j=j: inc(e.dma_start(out=stg[32:33, :], in_=brow_d[l, j:j + 1, :])),
                  "browst", writes=[kst0, kst1])
            DVE(lambda e, j=j: e.tensor_tensor(out=brow[32:33, j * 1024:(j + 1) * 1024], in0=stg[32:33, :],
                                               in1=browhi32[32:33, :], op=ALU.subtract),
                [kst0, kst1, ("pg", 21), "brow"], ["brow"])
        ACT(lambda e: e.activation(out=sptmp[:], in_=bpar[:, :, 7], func=AF.Exp, scale=-1.0), ["bpar"], ["sptmp"])
        ACT(lambda e: e.activation(out=sptmp[:], in_=sptmp[:], func=AF.Ln, bias=1.0), ["sptmp"], ["sptmp"])
        DVE(lambda e: e.tensor_scalar(out=negc[:], in0=sptmp[:], scalar1=-8.0, scalar2=None, op0=ALU.mult),
            ["sptmp"], ["negc"])
        DVE(lambda e: e.tensor_scalar(out=neg2c[:], in0=sptmp[:], scalar1=-16.0, scalar2=None, op0=ALU.mult),
            ["sptmp"], ["neg2c"])
        for ct in range(2):
            wtile, wk = wload(mat_view(w_kv_d, l, 0, 8, ct * 512, 512), 8, 512)
            for dcl in range(4):
                dc = ct * 4 + dcl
                ps, kp = nextps()

                def f(e, ps=ps, wtile=wtile, dcl=dcl):
                    last = None
                    for k in range(8):
                        last = e.matmul(ps[:, 0:256], lhsT=wtile[:, k, dcl * 128:(dcl + 1) * 128], rhs=memT[:, k, :],
                                        start=(k == 0), stop=(k == 7))
                    return last
                PE(f, [wk] + [("memT", k) for k in range(8)], [kp])
                ACT(lambda e, ps=ps, dc=dc: e.activation(out=KT[:, dc, :], in_=ps[:, 0:256], func=AF.Copy),
                    [kp], [("KT", dc)])
        for half in range(2):
            wtile, wk = wload(mat_view(w_kv_d, l, 0, 8, 1024 + half * 512, 512), 8, 512)
            for mc in range(2):
                ps, kp = nextps()

                def f(e, ps=ps, wtile=wtile, mc=mc):
                    last = None
                    for k in range(8):
                        last = e.matmul(ps[:], lhsT=memT[:, k, mc * 128:(mc + 1) * 128], rhs=wtile[:, k, :],
                                        start=(k == 0), stop=(k == 7))
                    return last
                PE(f, [wk] + [("memT", k) for k in range(8)], [kp])
                ACT(lambda e, ps=ps, mc=mc, half=half: e.activation(out=Vt[:, mc, half * 512:(half + 1) * 512],
                                                                    in_=ps[:], func=AF.Copy),
                    [kp], [("Vt", mc, half)])

    def cast_xb(blk):
        for k in range(8):
            s_ap, s_key = xs(k, blk)
            ACT(lambda e, k=k, s_ap=s_ap: e.activation(out=xb[:, k, :], in_=s_ap, func=AF.Copy), [s_key], [("xb", k)])

    XBK = [("xb", k) for k in range(8)]

    def mm_fm(ps, wtile, c0, K, rhs_of_k, extra=None):
        def f(e):
            last = None
            for k in range(K):
                last = e.matmul(ps[:], lhsT=wtile[:, k, c0:c0 + 128], rhs=rhs_of_k(k), start=(k == 0),
                                stop=(k == K - 1 and extra is None))
            if extra is not None:
                last = e.matmul(ps[:], lhsT=extra[0], rhs=extra[1], start=False, stop=True)
            return last
        return f

    def b_bufs(final):
        def pgs(lo, n):
            return [("pg", i) for i in range(lo, lo + n)]
        if final:
            sl = []
            xr2 = arena[:, 8 * 512:8 * 512 + 516]
            sl.append(dict(xr=XR[:, :], kxr=["XR"], xc=PG(17), xcb=HP(48), rr=PG(18), ii=PG(19), mm=PG(20), gg=HP(49)))
            sl.append(dict(xr=xr2, kxr=pgs(8, 2), xc=PG(10), xcb=HP(22), rr=PG(21), ii=PG(22), mm=PG(23), gg=HP(23)))
            return sl
        sl = []
        for i in range(4):
            if i == 0:
                xr, kxr = XR[:, :], ["XR"]
            else:
                base = (i - 1) * 2
                xr, kxr = arena[:, base * 512:base * 512 + 516], pgs(base, 2)
            sl.append(dict(xr=xr, kxr=kxr, xc=PG(6 + i), xcb=HP(20 + i), rr=PG(12 + i), ii=PG(16 + i), mm=PG(20 + i),
                           gg=None))
        return sl

    def b_branch(l, blk, final):
        sl = b_bufs(final)
        G = len(sl)
        st = {}
        def xproj(heads):
            pss = {}
            for h in heads:
                if h % 4 == 0:
                    ncol = min(512, 1280 - h * 128)
                    st["x"] = wload(win_view(l, 2048 + h * 128, ncol), 8, ncol)
                xtile, xk = st["x"]
                ps, kp = nextps(pin=True)
                PE(mm_fm(ps, xtile, (h % 4) * 128, 8, lambda k: xb[:, k, :]), [xk] + XBK, [kp])
                pss[h] = (ps, kp)
            return pss

        groups = [list(range(h0, min(10, h0 + G))) for h0 in range(0, 10, G)]
        pss_next = xproj(groups[0])
        for gi, heads in enumerate(groups):
            h0 = heads[0]
            pss = pss_next
            for h in heads:
                b = sl[h - h0]
                ps, kp = pss[h]
                xr, kxr = b["xr"], b["kxr"]
                xc, kxc = b["xc"]
                xcb, kxcb = b["xcb"]
                DVE(lambda e, xr=xr, h=h: e.tensor_copy(out=xr[:, 0:3], in_=HALO[:, h, :]), [("halo", h)], kxr)
                ACT(lambda e, xr=xr, ps=ps, h=h: e.activation(out=xr[:, 3:515], in_=ps[:], func=AF.Identity,
                                                               bias=binfm[:, 16 + h:17 + h]), [kp, "binfm"] + kxr, kxr)
                unpin(kp)
                DVE(lambda e, xr=xr, h=h: e.tensor_copy(out=HALO[:, h, :], in_=xr[:, 512:515]), kxr, [("halo", h)])
                DVE(lambda e, xr=xr, xc=xc, h=h: e.tensor_scalar(out=xc, in0=xr[:, 0:512], scalar1=bpar[:, h, 0:1],
                                                                 scalar2=bpar[:, h, 4:5], op0=ALU.mult, op1=ALU.add),
                    kxr + ["bpar"], [kxc])
                for tp in range(1, 4):
                    DVE(lambda e, xr=xr, xc=xc, h=h, tp=tp: e.scalar_tensor_tensor(
                        out=xc, in0=xr[:, tp:tp + 512], scalar=bpar[:, h, tp:tp + 1], in1=xc,
                        op0=ALU.mult, op1=ALU.add), kxr + ["bpar", kxc], [kxc])
                ACT(lambda e, xc=xc, xcb=xcb: e.activation(out=xcb, in_=xc, func=AF.Copy), [kxc], [kxcb])
            if gi + 1 < len(groups):
                pss_next = xproj(groups[gi + 1])
            psg = {}
            if final:
                for h in heads:
                    if h % 4 == 0:
                        ncol = min(512, 1280 - h * 128)
                        st["g"] = wload(win_view(l, 3328 + h * 128, ncol), 8, ncol)
                    gtile, gk = st["g"]
                    ps_g, kpg = nextps()
                    PE(mm_fm(ps_g, gtile, (h % 4) * 128, 8, lambda k: xb[:, k, :]), [gk] + XBK, [kpg])
                    psg[h] = (ps_g, kpg)
            for h in heads:
                b = sl[h - h0]
                xcb, kxcb = b["xcb"]
                ps_r, kpr = nextps()
                ps_i, kpi = nextps()
                PE(lambda e, ps_r=ps_r, xcb=xcb, h=h: e.matmul(ps_r[:], lhsT=waxb[:, 0, h, :], rhs=xcb, start=True,
                                                               stop=True), ["waxb", kxcb], [kpr])
                PE(lambda e, ps_i=ps_i, xcb=xcb, h=h: e.matmul(ps_i[:], lhsT=waxb[:, 1, h, :], rhs=xcb, start=True,
                                                               stop=True), ["waxb", kxcb], [kpi])
                rr, krr = b["rr"]
                ii, kii = b["ii"]
                ACT(lambda e, ps_r=ps_r, rr=rr, h=h: e.activation(out=rr, in_=ps_r[:], func=AF.Sigmoid,
                                                                  bias=bpar[:, h, 5:6]), [kpr, "bpar"], [krr])
                ACT(lambda e, ps_i=ps_i, ii=ii, h=h: e.activation(out=ii, in_=ps_i[:], func=AF.Sigmoid,
                                                                  bias=bpar[:, h, 6:7]), [kpi, "bpar"], [kii])
            for h in heads:
                b = sl[h - h0]
                rr, krr = b["rr"]
                mm, kmm = b["mm"]
                ACT(lambda e, rr=rr, mm=mm, h=h: e.activation(out=mm, in_=rr, func=AF.Exp, scale=neg2c[:, h:h + 1]),
                    [krr, "neg2c"], [kmm])
                ACT(lambda e, rr=rr, h=h: e.activation(out=rr, in_=rr, func=AF.Exp, scale=negc[:, h:h + 1]),
                    [krr, "negc"], [krr])
                DVE(lambda e, mm=mm: e.tensor_scalar(out=mm, in0=mm, scalar1=1.0, scalar2=-1.0, op0=ALU.min,
                                                     op1=ALU.mult), [kmm], [kmm])
            for h in heads:
                b = sl[h - h0]
                mm, kmm = b["mm"]
                ACT(lambda e, mm=mm: e.activation(out=mm, in_=mm, func=AF.Sqrt, bias=1.0), [kmm], [kmm])
            for h in heads:
                b = sl[h - h0]
                rr, krr = b["rr"]
                ii, kii = b["ii"]
                mm, kmm = b["mm"]
                xc, kxc = b["xc"]
                DVE(lambda e, ii=ii, xc=xc: e.tensor_tensor(out=ii, in0=ii, in1=xc, op=ALU.mult), [kii, kxc], [kii])
                DVE(lambda e, ii=ii, mm=mm: e.tensor_tensor(out=ii, in0=ii, in1=mm, op=ALU.mult), [kii, kmm], [kii])
                DVE(lambda e, xc=xc, rr=rr, ii=ii, h=h: e.tensor_tensor_scan(out=xc, data0=rr, data1=ii,
                                                                            initial=HC[:, h:h + 1], op0=ALU.mult,
                                                                            op1=ALU.add),
                    [krr, kii, ("hc", h), kxc], [kxc])
                DVE(lambda e, xc=xc, h=h: e.tensor_copy(out=HC[:, h:h + 1], in_=xc[:, 511:512]), [kxc], [("hc", h)])
            if final:
                for h in heads:
                    b = sl[h - h0]
                    gg, kgg = b["gg"]
                    ps_g, kpg = psg[h]
                    ACT(lambda e, ps_g=ps_g, gg=gg, h=h: e.activation(out=gg, in_=ps_g[:], func=AF.Gelu_apprx_tanh,
                                                                      bias=binfm[:, 26 + h:27 + h]),
                        [kpg, "binfm"], [kgg])
                for h in heads:
                    b = sl[h - h0]
                    gg, kgg = b["gg"]
                    xc, kxc = b["xc"]
                    ob, kob = HP(24 + h)
                    DVE(lambda e, ob=ob, xc=xc, gg=gg: e.tensor_tensor(out=ob, in0=xc, in1=gg, op=ALU.mult),
                        [kxc, kgg], [kob])
            yield

    def run_gen(g):
        for _ in g:
            pass

    def project_gate(l, o_tiles, pd, nK, gate_col0, first, last):
        for ct in range(2):
            if nK == 8:
                ptiles = [wload(mat_view(pd, l, 0, 8, ct * 512, 512), 8, 512)]
                kmap = [(0, k) for k in range(8)]
            else:
                ptiles = [wload(mat_view(pd, l, 0, 5, ct * 512, 512), 5, 512),
                          wload(mat_view(pd, l, 640, 5, ct * 512, 512), 5, 512)]
                kmap = [(0, k) for k in range(5)] + [(1, k) for k in range(5)]
            gtile, gk = wload(win_view(l, gate_col0 + ct * 512, 512), 8, 512)
            for dcl in range(4):
                dc = ct * 4 + dcl
                ps_g, kpg = nextps()
                PE(mm_fm(ps_g, gtile, dcl * 128, 8, lambda k: xb[:, k, :]), [gk] + XBK, [kpg])
                ps_p, kpp = nextps()

                def f(e, ps_p=ps_p, dcl=dcl, ptiles=ptiles):
                    lasti = None
                    for idx, (ti, kk) in enumerate(kmap):
                        lasti = e.matmul(ps_p[:], lhsT=ptiles[ti][0][:, kk, dcl * 128:(dcl + 1) * 128],
                                         rhs=o_tiles[idx][0], start=(idx == 0), stop=(idx == nK - 1))
                    return lasti
                PE(f, [t[1] for t in ptiles] + [t[1] for t in o_tiles], [kpp])
                gs, kgs = PG(17 + (dc % 2))
                ecol = gate_col0 // 128 + dc
                ACT(lambda e, ps_g=ps_g, gs=gs, ecol=ecol: e.activation(out=gs, in_=ps_g[:], func=AF.Sigmoid,
                                                                        bias=binfm[:, ecol:ecol + 1]),
                    [kpg, "binfm"], [kgs])
                y, ky = PG(dc)
                if first:
                    DVE(lambda e, y=y, ps_p=ps_p, gs=gs: e.tensor_tensor(out=y, in0=ps_p[:], in1=gs, op=ALU.mult),
                        [kpp, kgs], [ky])
                else:
                    DVE(lambda e, ps_p=ps_p, gs=gs: e.tensor_tensor(out=gs, in0=ps_p[:], in1=gs, op=ALU.mult),
                        [kpp, kgs], [kgs])
                    if not last:
                        DVE(lambda e, y=y, gs=gs: e.tensor_tensor(out=y, in0=y, in1=gs, op=ALU.add),
                            [ky, kgs], [ky])
                    else:
                        yb, kyb = HP(16 + dc)
                        DVE(lambda e, y=y, gs=gs, yb=yb: e.tensor_tensor(out=yb, in0=y, in1=gs, op=ALU.add),
                            [ky, kgs], [kyb])

    for l in range(n_layers):
        load_layer_consts(l)
        DVE(lambda e: e.memset(HC[:], 0.0), [], [("hc", h) for h in range(10)])
        DVE(lambda e: e.memset(HALO[:], 0.0), [], [("halo", h) for h in range(10)])
        for blk in range(NB):
            cast_xb(blk)
            run_gen(b_branch(l, blk, False))
        hck = [("hc", h) for h in range(10)]
        hak = [("halo", h) for h in range(10)]
        DVE(lambda e: e.tensor_copy(out=EX[:, 0:10], in_=HC[:]), hck, ["EX"])
        DVE(lambda e: e.tensor_copy(out=EX[:, 10:40], in_=HALO[:].rearrange("p h t -> p (h t)")), hak + ["EX"], ["EX"])
        S.dma("sp", lambda e, inc: inc(e.dma_start(out=cc_in, in_=EX[:])), "ccin", reads=["EX"], writes=["ccin"])
        POOL(lambda e: e.collective_compute("AllGather", ALU.bypass,
                                            replica_groups=[[0, 1], [2, 3], [4, 5], [6, 7]],
                                            ins=[cc_in], outs=[cc_out]), ["ccin"], ["ccout"])
        S.dma("sp", lambda e, inc: inc(e.dma_start(out=RX[:], in_=cc_out[0:128, :])), "rx", reads=["ccout"],
              writes=["RX"])
        DVE(lambda e: e.tensor_scalar(out=HC[:], in0=RX[:, 0:10], scalar1=flag[:, 0:1], scalar2=None, op0=ALU.mult),
            ["RX", "flag"], hck)
        DVE(lambda e: e.tensor_scalar(out=HALO[:].rearrange("p h t -> p (h t)"), in0=RX[:, 10:40],
                                      scalar1=flag[:, 0:1], scalar2=None, op0=ALU.mult), ["RX", "flag"], hak)

        branches = dbg[1] if (dbg and dbg[0] == "y") else "ABC"
        ydbg = bool(dbg and dbg[0] == "y")
        for blk in range(NB):
            cast_xb(blk)
            if dbg and dbg[0] == "x0":
                S.dma("sp", lambda e, inc, blk=blk: inc(e.dma_start(
                    out=out_d[:, blk * TB:(blk + 1) * TB].rearrange("(k p) t -> p k t", p=128),
                    in_=x32[:, :, blk * TB:(blk + 1) * TB])), "out%d" % blk,
                    reads=[("x32", k, blk) for k in range(8)], writes=[("out", blk)])
                continue
            if "A" in branches:
                for ct in range(2):
                    utile, uk = wload(win_view(l, ct * 512, 512), 8, 512)
                    for dcl in range(4):
                        dc = ct * 4 + dcl
                        ps, kp = nextps()
                        PE(mm_fm(ps, utile, dcl * 128, 8, lambda k: xb[:, k, :]), [uk] + XBK, [kp])
                        u_ap, u_key = HP(16 + dc)
                        ACT(lambda e, ps=ps, u_ap=u_ap, dc=dc: e.activation(out=u_ap, in_=ps[:], func=AF.Gelu_apprx_tanh,
                                                                            bias=binfm[:, dc:dc + 1]),
                            [kp, "binfm"], [u_key])
                vt0, vk0 = wload(win_view(l, 1024, 512), 8, 512)
                vt1, vk1 = wload(win_view(l, 1536, 512), 8, 512)
                def a_vmm(tc_):
                    pss = []
                    for half, (vt, vk) in enumerate(((vt0, vk0), (vt1, vk1))):
                        ps, kp = nextps()

                        def f(e, ps=ps, vt=vt, half=half, tc_=tc_):
                            for k in range(8):
                                e.matmul(ps[:], lhsT=xb[:, k, tc_ * 128:(tc_ + 1) * 128], rhs=vt[:, k, :],
                                         start=(k == 0), stop=False)
                            return e.matmul(ps[:], lhsT=ones33[:, 0:128], rhs=brow[:, half * 512:(half + 1) * 512],
                                            start=False, stop=True)
                        PE(f, [vk, "brow", "ones33"] + XBK, [kp])
                        pss.append((ps, kp))
                    return pss

                def a_chain(tc_, pss):
                    for half, (ps, kp) in enumerate(pss):
                        ACT(lambda e, ps=ps, half=half: e.activation(out=gv[:, half * 512:(half + 1) * 512], in_=ps[:],
                                                                      func=AF.Gelu_apprx_tanh,
                                                                      accum_out=stat[:, half:half + 1]),
                            [kp], [("gv", half), ("stat", half)])
                    for half in range(2):
                        ACT(lambda e, half=half: e.activation(out=PG(23 + half)[0],
                                                              in_=gv[:, half * 512:(half + 1) * 512], func=AF.Square,
                                                              accum_out=stat[:, 2 + half:3 + half]),
                            [("gv", half)], [PG(23 + half)[1], ("stat", 2 + half)])
                    stk = [("stat", i) for i in range(4)]
                    DVE(lambda e: e.tensor_tensor(out=stat[:, 4:5], in0=stat[:, 0:1], in1=stat[:, 1:2], op=ALU.add),
                        stk, ["st4"])
                    DVE(lambda e: e.tensor_tensor(out=stat[:, 5:6], in0=stat[:, 2:3], in1=stat[:, 3:4], op=ALU.add),
                        stk, ["st5"])
                    DVE(lambda e: e.tensor_scalar(out=stat[:, 4:6], in0=stat[:, 4:6], scalar1=1.0 / 1024.0, scalar2=None,
                                                  op0=ALU.mult), ["st4", "st5"], ["st4", "st5"])
                    DVE(lambda e: e.tensor_tensor(out=stat[:, 6:7], in0=stat[:, 4:5], in1=stat[:, 4:5], op=ALU.mult),
                        ["st4"], ["st6"])
                    DVE(lambda e: e.tensor_tensor(out=stat[:, 6:7], in0=stat[:, 5:6], in1=stat[:, 6:7], op=ALU.subtract),
                        ["st5", "st6"], ["st6"])
                    DVE(lambda e: e.tensor_scalar(out=stat[:, 6:7], in0=stat[:, 6:7], scalar1=0.0, scalar2=LN_EPS,
                                                  op0=ALU.max, op1=ALU.add), ["st6"], ["st6"])
                    ACT(lambda e: e.activation(out=stat[:, 6:7], in_=stat[:, 6:7], func=AF.Sqrt), ["st6"], ["st6"])
                    DVE(lambda e: e.reciprocal(out=stat[:, 7:8], in_=stat[:, 6:7]), ["st6"], ["st7"])
                    DVE(lambda e: e.scalar_tensor_tensor(out=stat[:, 8:9], in0=stat[:, 4:5], scalar=-1.0,
                                                         in1=stat[:, 7:8], op0=ALU.mult, op1=ALU.mult),
                        ["st4", "st7"], ["st8"])
                    for half in range(2):
                        sl = slice(half * 512, (half + 1) * 512)
                        DVE(lambda e, sl=sl: e.tensor_scalar(out=gv[:, sl], in0=gv[:, sl], scalar1=stat[:, 7:8],
                                                             scalar2=stat[:, 8:9], op0=ALU.mult, op1=ALU.add),
                            [("gv", half), "st7", "st8"], [("gv", half)])
                        DVE(lambda e, sl=sl: e.tensor_tensor(out=gv[:, sl], in0=gv[:, sl], in1=lnvbc[:, 0, sl],
                                                             op=ALU.mult), [("gv", half), "lnvg"], [("gv", half)])
                        DVE(lambda e, sl=sl: e.tensor_tensor(out=vn[:, sl], in0=gv[:, sl], in1=lnvbc[:, 1, sl],
                                                             op=ALU.add), [("gv", half), "lnvb"], [("vn", half)])

                def a_mix(tc_):
                    for gh in range(2):
                        ps, kp = nextps()

                        def f(e, ps=ps, gh=gh):
                            last = None
                            for gl in range(4):
                                g = gh * 4 + gl
                                e.matmul(ps[:, gl * 128:(gl + 1) * 128], lhsT=vn[:, g * 128:(g + 1) * 128],
                                         rhs=WmT[:, g, :], start=True, stop=False)
                                last = e.matmul(ps[:, gl * 128:(gl + 1) * 128], lhsT=ones33[:, 0:128],
                                                rhs=brow[:, 1024 + g * 128:1024 + (g + 1) * 128], start=False, stop=True)
                            return last
                        PE(f, [("vn", gh), "WmT", "brow", "ones33"], [kp])
                        for gl in range(4):
                            g = gh * 4 + gl
                            u_ap, u_key = HP(16 + g)
                            DVE(lambda e, ps=ps, gl=gl, u_ap=u_ap, tc_=tc_: e.tensor_tensor(
                                out=u_ap[:, tc_ * 128:(tc_ + 1) * 128], in0=ps[:, gl * 128:(gl + 1) * 128],
                                in1=u_ap[:, tc_ * 128:(tc_ + 1) * 128], op=ALU.mult), [kp, u_key], [u_key])

                pss_cur = a_vmm(0)
                a_chain(0, pss_cur)
                for tc_ in range(4):
                    if tc_ + 1 < 4:
                        pss_nxt = a_vmm(tc_ + 1)
                    a_mix(tc_)
                    if tc_ + 1 < 4:
                        a_chain(tc_ + 1, pss_nxt)
                project_gate(l, [HP(16 + k) for k in range(8)], p_a_d, 8, 5632, True, False)

            if "B" in branches:
                run_gen(b_branch(l, blk, True))
                project_gate(l, [HP(24 + h) for h in range(10)], p_b_d, 10, 5632 + 1024, branches[0] == 'B', False)

            if "C" in branches:
                for ct in range(2):
                    qtile, qk = wload(win_view(l, 4608 + ct * 512, 512), 8, 512)
                    for dcl in range(4):
                        dc = ct * 4 + dcl
                        ps, kp = nextps()
                        PE(mm_fm(ps, qtile, dcl * 128, 8, lambda k: xb[:, k, :]), [qk] + XBK, [kp])
                        q_ap, q_key = HP(34 + dc)
                        ACT(lambda e, ps=ps, q_ap=q_ap, dc=dc: e.activation(out=q_ap, in_=ps[:], func=AF.Identity,
                                                                            bias=binfm[:, 36 + dc:37 + dc]),
                            [kp, "binfm"], [q_key])
                for hd in range(4):
                    ekeys = []
                    for mc in range(2):
                        ps, kp = nextps()

                        def f(e, ps=ps, hd=hd, mc=mc):
                            e.matmul(ps[:], lhsT=KT[:, hd * 2, mc * 128:(mc + 1) * 128], rhs=HP(34 + hd * 2)[0],
                                     start=True, stop=False)
                            return e.matmul(ps[:], lhsT=KT[:, hd * 2 + 1, mc * 128:(mc + 1) * 128],
                                            rhs=HP(34 + hd * 2 + 1)[0], start=False, stop=True)
                        PE(f, [("KT", hd * 2), ("KT", hd * 2 + 1), HP(34 + hd * 2)[1], HP(34 + hd * 2 + 1)[1]], [kp])
                        ei = (hd % 2) * 2 + mc
                        ACT(lambda e, ps=ps, ei=ei: e.activation(out=HP(24 + ei)[0], in_=ps[:], func=AF.Exp, scale=1.0 / 16.0),
                            [kp], [HP(24 + ei)[1]])
                        ekeys.append(HP(24 + ei)[1])
                    e0 = (hd % 2) * 2
                    ps_d, kpd = nextps()
                    PE(lambda e, ps_d=ps_d, e0=e0: (e.matmul(ps_d[:], lhsT=onesB[:], rhs=HP(24 + e0)[0], start=True, stop=False),
                                                     e.matmul(ps_d[:], lhsT=onesB[:], rhs=HP(24 + e0 + 1)[0], start=False,
                                                              stop=True))[1], ekeys + ["onesB"], [kpd])
                    rden, krd = PG(14 + (hd % 2))
                    DVE(lambda e, ps_d=ps_d, rden=rden: e.reciprocal(out=rden, in_=ps_d[:]), [kpd], [krd])
                    for dl in range(2):
                        ps_o, kpo = nextps()
                        c0 = hd * 256 + dl * 128
                        PE(lambda e, ps_o=ps_o, c0=c0, e0=e0: (
                            e.matmul(ps_o[:], lhsT=Vt[:, 0, c0:c0 + 128], rhs=HP(24 + e0)[0], start=True, stop=False),
                            e.matmul(ps_o[:], lhsT=Vt[:, 1, c0:c0 + 128], rhs=HP(24 + e0 + 1)[0], start=False, stop=True))[1],
                           ekeys + [("Vt", 0, c0 // 512), ("Vt", 1, c0 // 512)], [kpo])
                        oc, koc = HP(42 + hd * 2 + dl)
                        DVE(lambda e, ps_o=ps_o, oc=oc, rden=rden: e.tensor_tensor(out=oc, in0=ps_o[:], in1=rden,
                                                                                    op=ALU.mult), [kpo, krd], [koc])
                project_gate(l, [HP(42 + k) for k in range(8)], p_c_d, 8, 5632 + 2048, branches[0] == 'C', not ydbg)

            if ydbg:
                S.dma("sp", lambda e, inc, blk=blk: inc(e.dma_start(
                    out=out_d[:, blk * TB:(blk + 1) * TB].rearrange("(k p) t -> p k t", p=128),
                    in_=arena[:, 0:4096].rearrange("p (k t) -> p k t", k=8))), "out%d" % blk,
                    reads=[("pg", k) for k in range(8)], writes=[("out", blk)])
                continue
            for ct in range(2):
                otile, ok_ = wload(mat_view(w_o_d, l, 0, 8, ct * 512, 512), 8, 512)
                for dcl in range(4):
                    dc = ct * 4 + dcl
                    ps, kp = nextps()
                    PE(mm_fm(ps, otile, dcl * 128, 8, lambda k: HP(16 + k)[0],
                             extra=(brow[:, 2048 + dc * 128:2048 + (dc + 1) * 128], ones33[:, :])),
                       [ok_, "brow", "ones33"] + [HP(16 + k)[1] for k in range(8)], [kp])
                    x_ap, x_key = xs(dc, blk)
                    DVE(lambda e, ps=ps, x_ap=x_ap: e.scalar_tensor_tensor(out=x_ap, in0=x_ap, scalar=ALPHA,
                                                                            in1=ps[:], op0=ALU.mult, op1=ALU.add),
                        [kp, x_key], [x_key])
            ln_fm(lambda k, blk=blk: xs(k, blk), lambda k, blk=blk: xs(k, blk), xbk,
                  lambda k: lnp[:, k, 0:1], lambda k: lnp[:, k, 1:2], TB, "lnp")

            if dbg and dbg[0] == "x1":
                S.dma("sp", lambda e, inc, blk=blk: inc(e.dma_start(
                    out=out_d[:, blk * TB:(blk + 1) * TB].rearrange("(k p) t -> p k t", p=128),
                    in_=x32[:, :, blk * TB:(blk + 1) * TB])), "out%d" % blk,
                    reads=[("x32", k, blk) for k in range(8)], writes=[("out", blk)])
                continue
            L = lambda c, a, b: rt[:, c, a:b]
            for c in range(4):
                ps, kp = nextps()

                def f(e, ps=ps, c=c, blk=blk):
                    for k in range(8):
                        e.matmul(ps[:, 0:20], lhsT=x32[:, k, blk * TB + c * 128:blk * TB + (c + 1) * 128],
                                 rhs=w_r[:, k, :], start=(k == 0), stop=False)
                    return e.matmul(ps[:, 0:20], lhsT=ones32r[:, :], rhs=b_r[:, :], start=False, stop=True)
                PE(f, [("x32", k, blk) for k in range(8)] + ["w_r", "b_r", "ones32r"], [kp])
                rk = ("rt", c)
                DVE(lambda e, ps=ps, c=c: e.tensor_copy(out=L(c, 0, 20), in_=ps[:, 0:20]), [kp], [rk])
                DVE(lambda e, c=c: e.tensor_reduce(out=L(c, 20, 21), in_=L(c, 0, 4), axis=AX.X, op=ALU.max), [rk], [rk])
                DVE(lambda e, c=c: e.tensor_scalar(out=L(c, 21, 22), in0=L(c, 20, 21), scalar1=-1.0, scalar2=None,
                                                   op0=ALU.mult), [rk], [rk])
                DVE(lambda e, c=c: e.tensor_scalar(out=L(c, 24, 28), in0=L(c, 0, 4), scalar1=L(c, 20, 21),
                                                   scalar2=None, op0=ALU.is_ge), [rk], [rk])
                DVE(lambda e, c=c: e.tensor_scalar(out=L(c, 24, 28), in0=L(c, 24, 28), scalar1=-1.0, scalar2=-NEG,
                                                   op0=ALU.add, op1=ALU.mult), [rk], [rk])
                for g in range(4):
                    DVE(lambda e, c=c, g=g: e.tensor_scalar(out=L(c, 28 + 4 * g, 32 + 4 * g),
                                                            in0=L(c, 4 + 4 * g, 8 + 4 * g),
                                                            scalar1=L(c, 24 + g, 25 + g), scalar2=None, op0=ALU.add),
                        [rk], [rk])
                DVE(lambda e, c=c: e.tensor_reduce(out=L(c, 44, 45), in_=L(c, 28, 44), axis=AX.X, op=ALU.max), [rk], [rk])
                DVE(lambda e, c=c: e.tensor_scalar(out=L(c, 45, 46), in0=L(c, 44, 45), scalar1=-1.0, scalar2=None,
                                                   op0=ALU.mult), [rk], [rk])
                DVE(lambda e, c=c: e.tensor_scalar(out=L(c, 48, 64), in0=L(c, 28, 44), scalar1=L(c, 44, 45),
                                                   scalar2=NEG, op0=ALU.is_ge, op1=ALU.mult), [rk], [rk])
                DVE(lambda e, c=c: e.tensor_tensor(out=L(c, 48, 64), in0=L(c, 48, 64), in1=L(c, 28, 44), op=ALU.add),
                    [rk], [rk])
                DVE(lambda e, c=c: e.tensor_reduce(out=L(c, 46, 47), in_=L(c, 48, 64), axis=AX.X, op=ALU.max), [rk], [rk])
                DVE(lambda e, c=c: e.tensor_scalar(out=L(c, 80, 96), in0=L(c, 28, 44), scalar1=L(c, 46, 47),
                                                   scalar2=None, op0=ALU.is_ge), [rk], [rk])
                DVE(lambda e, c=c: e.tensor_tensor(out=L(c, 47, 48), in0=L(c, 46, 47), in1=L(c, 44, 45),
                                                   op=ALU.subtract), [rk], [rk])
            for c in range(4):
                rk = ("rt", c)
                ACT(lambda e, c=c: e.activation(out=L(c, 64, 68), in_=L(c, 0, 4), func=AF.Exp, bias=L(c, 21, 22),
                                                accum_out=L(c, 22, 23)), [rk], [rk])
                ACT(lambda e, c=c: e.activation(out=L(c, 64, 80), in_=L(c, 28, 44), func=AF.Exp, bias=L(c, 45, 46)),
                    [rk], [rk])
                ACT(lambda e, c=c: e.activation(out=L(c, 47, 48), in_=L(c, 47, 48), func=AF.Exp), [rk], [rk])
            for c in range(4):
                rk = ("rt", c)
                DVE(lambda e, c=c: e.tensor_scalar(out=L(c, 47, 48), in0=L(c, 47, 48), scalar1=1.0, scalar2=None,
                                                   op0=ALU.add), [rk], [rk])
                DVE(lambda e, c=c: e.tensor_tensor(out=L(c, 47, 48), in0=L(c, 47, 48), in1=L(c, 22, 23), op=ALU.mult),
                    [rk], [rk])
                DVE(lambda e, c=c: e.reciprocal(out=L(c, 23, 24), in_=L(c, 47, 48)), [rk], [rk])
                DVE(lambda e, c=c: e.scalar_tensor_tensor(out=L(c, 80, 96), in0=L(c, 64, 80), scalar=L(c, 23, 24),
                                                          in1=L(c, 80, 96), op0=ALU.mult, op1=ALU.mult), [rk], [rk])
                DVE(lambda e, c=c: e.tensor_copy(out=G2[:, c, 0:16], in_=L(c, 80, 96)), [rk], [("G2", c)])
                DVE(lambda e, c=c: e.tensor_tensor(out=G2[:, c, 16:32], in0=L(c, 80, 96), in1=G2[:, c, 0:16],
                                                   op=ALU.subtract), [rk, ("G2", c)], [("G2", c)])
            gT2, kgT2 = HP(42)

            def emit_transpose():
                psT, kpT = nextps()
                psTb = psT[:].bitcast(BF16)
                PE(lambda e: [e.transpose(out=psTb[0:32, c * 128:(c + 1) * 128], in_=G2[:, c, :], identity=identb[:])
                              for c in range(4)][-1], [("G2", c) for c in range(4)] + ["identb"], [kpT])
                ACT(lambda e: e.activation(out=gT2[0:32, :], in_=psTb[0:32, 0:512], func=AF.Copy), [kpT], [kgT2])

            def emit_gb_h(ex, tl):
                ps_gb, kgb = nextps()
                PE(lambda e: e.matmul(ps_gb[:], lhsT=SEL2[:, ex, :], rhs=gT2[0:32, :], start=True, stop=True),
                   ["SEL2", kgT2], [kgb])
                for j, (t_ap, t_key) in enumerate(tl):
                    h_ap, h_key = HP(ex * 2 + j)
                    DVE(lambda e, t_ap=t_ap, h_ap=h_ap: e.tensor_tensor(out=h_ap, in0=ps_gb[:], in1=t_ap, op=ALU.mult),
                        [kgb, t_key], [h_key])

            pending = []
            for ex in range(16):
                wu, wuk = wload(w_up_d[l, ex].rearrange("(k p) c -> p k c", p=128), 8, 512)
                tl = []
                for j in range(2):
                    ps_g, kpg = nextps()
                    ps_u, kpu = nextps()
                    PE(mm_fm(ps_g, wu, j * 128, 8, lambda k: xb[:, k, :]), [wuk] + XBK, [kpg])
                    PE(mm_fm(ps_u, wu, 256 + j * 128, 8, lambda k: xb[:, k, :]), [wuk] + XBK, [kpu])
                    s_ap, s_key = PG(17 + j)
                    t_ap, t_key = PG((19 if ex % 2 == 0 else 22) + j)
                    ACT(lambda e, ps_g=ps_g, s_ap=s_ap: e.activation(out=s_ap, in_=ps_g[:], func=AF.Silu),
                        [kpg], [s_key])
                    DVE(lambda e, ps_u=ps_u, s_ap=s_ap, t_ap=t_ap: e.tensor_tensor(out=t_ap, in0=ps_u[:], in1=s_ap,
                                                                                    op=ALU.mult),
                        [kpu, s_key], [t_key])
                    tl.append((t_ap, t_key))
                pending.append((ex, tl))
                if ex == 1:
                    emit_transpose()
                if ex >= 1:
                    for p in pending:
                        emit_gb_h(*p)
                    pending = []
            for dc in range(8):
                s = w_ctr[0] % NSLOT
                w_ctr[0] += 1
                wd = wt[s][:, :].rearrange("p (i d) -> p i d", i=32)
                wdk = ("w", s)
                S.dma("pool", lambda e, inc, s=s, dc=dc, l=l: inc(e.dma_start(out=wt[s][:, :], in_=w_dn_d[l, dc])),
                      "w%d" % s, writes=[wdk])
                ps, kp = nextps()

                def f(e, ps=ps, wd=wd):
                    last = None
                    for i in range(32):
                        last = e.matmul(ps[:], lhsT=wd[:, i, :], rhs=HP(i)[0], start=(i == 0), stop=(i == 31))
                    return last
                PE(f, [wdk] + [("pg", i) for i in range(16)], [kp])
                x_ap, x_key = xs(dc, blk)
                DVE(lambda e, ps=ps, x_ap=x_ap: e.scalar_tensor_tensor(out=x_ap, in0=x_ap, scalar=ALPHA, in1=ps[:],
                                                                        op0=ALU.mult, op1=ALU.add),
                    [kp, x_key], [x_key])
            ln_fm(lambda k, blk=blk: xs(k, blk), lambda k, blk=blk: xs(k, blk), None,
                  lambda k: lnp[:, k, 2:3], lambda k: lnp[:, k, 3:4], TB, "lnp")
            if l == n_layers - 1:
                S.dma("sp", lambda e, inc, blk=blk: inc(e.dma_start(
                    out=out_d[:, blk * TB:(blk + 1) * TB].rearrange("(k p) t -> p k t", p=128),
                    in_=x32[:, :, blk * TB:(blk + 1) * TB])), "out%d" % blk,
                    reads=[("x32", k, blk) for k in range(8)], writes=[("out", blk)])
    S.wait_all("sp", [("out", blk) for blk in range(NB)])
    S.emit()
    S.close()
    return nc


def _fm(v):
    return np.ascontiguousarray(v.reshape(8, 128).T)


def prep_inputs(inp):
    f = lambda a: np.ascontiguousarray(np.asarray(a, dtype=np.float32))
    D = DEPTH
    shared = {}
    shared["ln0"] = f(np.stack([_fm(inp["ln_in_g"]), _fm(inp["ln_in_b"]), _fm(inp["ln_mem_g"]), _fm(inp["ln_mem_b"])], axis=-1))
    shared["w_in"] = f(inp["w_in"])
    shared["bin_fm"] = f(np.stack([np.asarray(inp["b_in"][l]).reshape(68, 128).T for l in range(D)]))
    shared["brow"] = f(np.stack([np.stack([np.asarray(inp["b_in"][l][1024:2048]), np.asarray(inp["b_s"][l]).reshape(1024),
                                           np.asarray(inp["b_o"][l])]) for l in range(D)]))
    shared["lnv"] = f(np.stack([np.stack([inp["ln_v_g"][l], inp["ln_v_b"][l]]) for l in range(D)]))
    shared["wsT"] = f(np.stack([np.asarray(inp["w_s"][l]).transpose(2, 0, 1) for l in range(D)]))
    bp = []
    for l in range(D):
        cw = np.asarray(inp["conv_w"][l]).reshape(4, 10, 128).transpose(2, 1, 0)
        others = [np.asarray(inp[n][l]).reshape(10, 128).T[:, :, None] for n in ("conv_b", "b_a", "b_x", "lam")]
        bp.append(np.concatenate([cw] + others, axis=-1))
    shared["bpar"] = f(np.stack(bp))
    shared["wax"] = f(np.stack([np.stack([inp["w_a"][l], inp["w_x"][l]]) for l in range(D)]))
    shared["w_kv"] = f(inp["w_kv"])
    shared["p_a"] = f(inp["p_a"])
    shared["p_b"] = f(inp["p_b"])
    shared["p_c"] = f(inp["p_c"])
    shared["w_o"] = f(inp["w_o"])
    shared["lnp"] = f(np.stack([np.stack([_fm(inp["ln1_g"][l]), _fm(inp["ln1_b"][l]), _fm(inp["ln2_g"][l]),
                                          _fm(inp["ln2_b"][l])], axis=-1) for l in range(D)]))
    wr = []
    for l in range(D):
        w = np.concatenate([np.asarray(inp["w_rg"][l]), np.asarray(inp["w_re"][l])], axis=1)
        wr.append(w.reshape(8, 128, 20).transpose(1, 0, 2))
    shared["w_r"] = f(np.stack(wr))
    shared["b_r"] = f(np.stack([np.concatenate([np.asarray(inp["b_rg"][l]), np.asarray(inp["b_re"][l])])[None, :]
                                for l in range(D)]))
    shared["w_up"] = f(inp["w_up"])
    wd = []
    for l in range(D):
        w = np.asarray(inp["w_down"][l]).reshape(16, 2, 128, 8, 128)
        wd.append(w.transpose(3, 2, 0, 1, 4).reshape(8, 128, 32 * 128))
    shared["w_dn"] = f(np.stack(wd))
    x = np.asarray(inp["x"], dtype=np.float32)
    mem = np.asarray(inp["mem"], dtype=np.float32)
    in_maps = []
    for c in range(NCORES):
        b, hh = c // 2, c % 2
        m = dict(shared)
        m["xT"] = np.ascontiguousarray(x[b, hh * NT:(hh + 1) * NT, :].T)
        m["memT"] = np.ascontiguousarray(mem[b].T)
        m["flag"] = np.full((128, 1), float(hh), np.float32)
        in_maps.append(m)
    return in_maps


_NC_CACHE = {}


def kernel(_dbg=None, _nl=DEPTH, **inputs):
    in_maps = prep_inputs(inputs)
    if _dbg is not None or _nl != DEPTH:
        _NC_CACHE["nc"] = build_program(_nl, _dbg)
    if "nc" not in _NC_CACHE:
        _NC_CACHE["nc"] = build_program()
    nc = _NC_CACHE["nc"]
    res = run_bass_kernel_spmd(nc, in_maps, core_ids=list(range(NCORES)))
    out = np.empty((4, 4096, 1024), np.float32)
    for c in range(NCORES):
        b, hh = c // 2, c % 2
        out[b, hh * NT:(hh + 1) * NT, :] = res.results[c]["out"].T
    return out
```

```python
import numpy as np
import concourse.bass as bass
import concourse.mybir as mybir
from concourse.bass_utils import run_bass_kernel_spmd

F32 = mybir.dt.float32
BF16 = mybir.dt.bfloat16
AF = mybir.ActivationFunctionType
ALU = mybir.AluOpType
AX = mybir.AxisListType

ENGS = ("pe", "act", "dve", "pool", "sp")

DEPTH = 2
NCORES = 8
NT = 2048
TB = 512
NB = NT // TB
ALPHA = float((2 * DEPTH) ** 0.25)
LN_EPS = 1e-5
NEG = -1.0e30
NO_SAME_ENGINE_SYNC = ("pe", "sp")


class Sched:
    def __init__(self, nc):
        self.nc = nc
        self.ops = {e: [] for e in ENGS}
        self.cnt = {e: 0 for e in ENGS}
        self.sem = {}
        self.dma_cnt = {}
        self.last_w = {}
        self.readers = {}
        self.seen = {e: {} for e in ENGS}
        self._ctx = []

    def _get_sem(self, name):
        if name not in self.sem:
            cm = self.nc.semaphore(name)
            s = cm.__enter__()
            self._ctx.append(cm)
            self.sem[name] = s
        return self.sem[name]

    def _waits_for(self, eng, reads, writes, same_engine_raw=True):
        need = {}

        def add(ev):
            if ev is None:
                return
            sname, val, weng = ev
            if weng == eng and (not same_engine_raw or eng in NO_SAME_ENGINE_SYNC):
                return
            if need.get(sname, 0) < val:
                need[sname] = val

        for k in reads:
            add(self.last_w.get(k))
        for k in writes:
            add(self.last_w.get(k))
            for ev in self.readers.get(k, []):
                if ev[2] == eng:
                    continue
                add(ev)
        out = []
        for sname, val in need.items():
            if self.seen[eng].get(sname, 0) >= val:
                continue
            self.seen[eng][sname] = val
            out.append((sname, val))
        return out

    def _record(self, ev, reads, writes):
        for k in reads:
            self.readers.setdefault(k, []).append(ev)
        for k in writes:
            self.last_w[k] = ev
            self.readers[k] = []

    def op(self, eng, fn, reads=(), writes=()):
        waits = self._waits_for(eng, reads, writes)
        self.cnt[eng] += 1
        ev = ("prog_" + eng, self.cnt[eng], eng)
        self._get_sem(ev[0])
        for s, _ in waits:
            self._get_sem(s)
        self.ops[eng].append(("op", fn, waits, ev))
        self._record(ev, reads, writes)
        return ev

    def dma(self, eng, fn, key, n=1, reads=(), writes=()):
        waits = self._waits_for(eng, reads, writes, same_engine_raw=True)
        sname = "dma_" + str(key)
        self._get_sem(sname)
        self.dma_cnt[sname] = self.dma_cnt.get(sname, 0) + 16 * n
        ev = (sname, self.dma_cnt[sname], "dma:" + str(key))
        for s, _ in waits:
            self._get_sem(s)
        self.ops[eng].append(("dma", fn, waits, ev))
        self._record(ev, reads, writes)
        return ev

    def wait_all(self, eng, keys):
        waits = self._waits_for(eng, keys, (), same_engine_raw=False)
        self.ops[eng].append(("wait", None, waits, None))

    def emit(self):
        nc = self.nc
        sem = self.sem
        ops = self.ops

        def replay(e, name):
            for kind, fn, waits, ev in ops[name]:
                for s, v in waits:
                    e.wait_ge(sem[s], v)
                if kind == "op":
                    ins = fn(e)
                    ins.then_inc(sem[ev[0]], 1)
                elif kind == "dma":
                    s = sem[ev[0]]
                    fn(e, lambda ins, s=s: ins.then_inc(s, 16))

        with nc.Block() as block:
            @block.tensor
            def _(e):
                replay(e, "pe")

            @block.scalar
            def _(e):
                replay(e, "act")

            @block.vector
            def _(e):
                replay(e, "dve")

            @block.gpsimd
            def _(e):
                replay(e, "pool")

            @block.sync
            def _(e):
                replay(e, "sp")

    def close(self):
        for cm in reversed(self._ctx):
            cm.__exit__(None, None, None)


def build_program(n_layers=DEPTH, dbg=None):
    nc = bass.Bass("TRN2", target_bir_lowering=False)
    S = Sched(nc)

    def din(name, shape, dt=F32):
        return nc.dram_tensor(name, list(shape), dt, kind="ExternalInput").ap()

    xT_d = din("xT", [1024, NT])
    memT_d = din("memT", [1024, 256])
    flag_d = din("flag", [128, 1])
    ln0_d = din("ln0", [128, 8, 4])
    w_in_d = din("w_in", [DEPTH, 1024, 8704])
    binfm_d = din("bin_fm", [DEPTH, 128, 68])
    brow_d = din("brow", [DEPTH, 3, 1024])
    lnv_d = din("lnv", [DEPTH, 2, 1024])
    wsT_d = din("wsT", [DEPTH, 128, 8, 128])
    bpar_d = din("bpar", [DEPTH, 128, 10, 8])
    wax_d = din("wax", [DEPTH, 2, 10, 128, 128])
    w_kv_d = din("w_kv", [DEPTH, 1024, 2048])
    p_a_d = din("p_a", [DEPTH, 1024, 1024])
    p_b_d = din("p_b", [DEPTH, 1280, 1024])
    p_c_d = din("p_c", [DEPTH, 1024, 1024])
    w_o_d = din("w_o", [DEPTH, 1024, 1024])
    lnp_d = din("lnp", [DEPTH, 128, 8, 4])
    w_r_d = din("w_r", [DEPTH, 128, 8, 20])
    b_r_d = din("b_r", [DEPTH, 1, 20])
    w_up_d = din("w_up", [DEPTH, 16, 1024, 512])
    w_dn_d = din("w_dn", [DEPTH, 8, 128, 32 * 128])
    out_d = nc.dram_tensor("out", [1024, NT], F32, kind="ExternalOutput").ap()
    cc_in1 = nc.dram_tensor("cc_in1", [128, 30], F32).ap()
    cc_out1 = nc.dram_tensor("cc_out1", [256, 30], F32).ap()
    cc_in2 = nc.dram_tensor("cc_in2", [128, 10], F32).ap()
    cc_out2 = nc.dram_tensor("cc_out2", [256, 10], F32).ap()
    spill_d = nc.dram_tensor("spill", [NB, 10, 128, 1024], BF16).ap()

    def sb(name, shape, dt):
        return nc.alloc_sbuf_tensor("s_" + name, list(shape), dt)

    x32 = sb("x32", [128, 8, NT], F32)
    xb = sb("xb", [128, 8, TB], BF16)
    NPG = 25
    arena = sb("arena", [128, NPG * 512], F32)
    arena_bf = arena[:].bitcast(BF16)

    def PG(i):
        return arena[:, i * 512:(i + 1) * 512], ("pg", i)

    def HP(j):
        return arena_bf[:, j * 512:(j + 1) * 512], ("pg", j // 2)

    gv = sb("gv", [128, 1024], F32)
    vn = sb("vn", [128, 1024], BF16)
    XR = sb("XR", [128, 516], F32)
    NSLOT = 4
    wt = [sb("wt%d" % i, [128, 8 * 512], BF16) for i in range(NSLOT)]
    KT = sb("KT", [128, 8, 256], BF16)
    Vt = sb("Vt", [128, 2, 1024], BF16)
    memT = sb("memTb", [128, 8, 256], BF16)
    lnvbc = sb("lnvbc", [128, 2, 1024], F32)
    WmT = sb("WmT", [128, 8, 128], BF16)
    waxb = sb("waxb", [128, 2, 10, 128], BF16)
    brow = sb("browb", [33, 3072], BF16)
    w_r = sb("w_r", [128, 8, 20], F32)
    b_r = sb("b_r", [1, 20], F32)
    bpar = sb("bpar", [128, 10, 8], F32)
    negc = sb("negc", [128, 10], F32)
    neg2c = sb("neg2c", [128, 10], F32)
    sptmp = sb("sptmp", [128, 10], F32)
    lnp = sb("lnp", [128, 8, 4], F32)
    ln0 = sb("ln0", [128, 8, 4], F32)
    binfm = sb("binfm", [128, 68], F32)
    identb = sb("identb", [128, 128], BF16)
    onesS = sb("onesS", [128, 128], BF16)
    onesB = sb("onesB", [128, 128], BF16)
    ones33 = sb("ones33", [33, 512], BF16)
    ones32r = sb("ones32r", [1, 128], F32)
    SEL2 = sb("SEL2", [32, 16, 128], BF16)
    flag = sb("flag", [128, 1], F32)
    HC = sb("HC", [128, 10], F32)
    HALO = sb("HALO", [128, 10, 3], F32)
    EX = sb("EX", [128, 40], F32)
    PC = sb("PC", [128, 10], F32)
    HCI = sb("HCI", [128, 10], F32)
    RX = sb("RX", [128, 40], F32)
    stat = sb("stat", [128, 16], F32)
    rt = sb("rt", [128, 4, 96], F32)
    G2 = sb("G2", [128, 4, 32], BF16)
    zero512 = None

    psb = [nc.alloc_psum_tensor("ps%d" % i, [128, 512], F32) for i in range(8)]
    ps_ctr = [0]

    ps_pinned = set()

    def nextps(pin=False):
        assert len(ps_pinned) < 8, "all PSUM banks pinned"
        while True:
            i = ps_ctr[0] % 8
            ps_ctr[0] += 1
            if i not in ps_pinned:
                break
        if pin:
            ps_pinned.add(i)
        return psb[i], ("ps", i)

    def unpin(key):
        ps_pinned.discard(key[1])

    def PE(fn, r, w):
        return S.op("pe", fn, reads=r, writes=w)

    def ACT(fn, r, w):
        return S.op("act", fn, reads=r, writes=w)

    def DVE(fn, r, w):
        return S.op("dve", fn, reads=r, writes=w)

    def POOL(fn, r, w):
        return S.op("pool", fn, reads=r, writes=w)

    def LOAD(dst, src, key, q="sp"):
        S.dma(q, lambda e, inc: inc(e.dma_start(out=dst, in_=src)), key, writes=[key])

    w_ctr = [0]

    def wload(src_view, K, C):
        s = w_ctr[0] % NSLOT
        w_ctr[0] += 1
        dst = wt[s][:, 0:K * C].rearrange("p (k c) -> p k c", k=K)
        key = ("w", s)
        S.dma("pool", lambda e, inc: inc(e.dma_start(out=dst, in_=src_view)), "w%d" % s, writes=[key])
        return dst, key

    def win_view(l, c0, C):
        return w_in_d[l, :, c0:c0 + C].rearrange("(k p) c -> p k c", p=128)

    def mat_view(d, l, r0, K, c0, C):
        return d[l, r0:r0 + K * 128, c0:c0 + C].rearrange("(k p) c -> p k c", p=128)

    POOL(lambda e: e.memset(onesS[:], 1.0 / 1024.0), [], ["onesS"])
    POOL(lambda e: e.memset(onesB[:], 1.0), [], ["onesB"])
    POOL(lambda e: e.memset(ones33[:], 1.0), [], ["ones33"])
    POOL(lambda e: e.memset(ones32r[:], 1.0), [], ["ones32r"])
    onesrc = arena[:, 12 * 512:13 * 512]
    POOL(lambda e: e.memset(arena[:, 12 * 512:16 * 512], 1.0), [], [("pg", 12), ("pg", 13), ("pg", 14), ("pg", 15)])
    selA = arena[0:32, 0:2048].rearrange("p (a m) -> p a m", a=16)
    selB = arena[0:32, 2048:4096].rearrange("p (a m) -> p a m", a=16)
    onesel = arena[0:32, 12 * 512:16 * 512].rearrange("p (a m) -> p a m", a=16)
    POOL(lambda e: e.affine_select(out=selA, in_=onesel, pattern=[[-1, 16], [0, 128]],
                                   compare_op=ALU.is_equal, fill=0.0, base=0, channel_multiplier=1),
         [("pg", i) for i in range(12, 16)], [("pg", i) for i in range(0, 4)])
    POOL(lambda e: e.affine_select(out=selB, in_=onesel, pattern=[[-1, 16], [0, 128]],
                                   compare_op=ALU.is_equal, fill=0.0, base=-16, channel_multiplier=1),
         [("pg", i) for i in range(12, 16)], [("pg", i) for i in range(4, 8)])
    POOL(lambda e: e.tensor_tensor(out=SEL2[:], in0=selA, in1=selB, op=ALU.add),
         [("pg", i) for i in range(0, 8)], ["SEL2"])
    POOL(lambda e: e.affine_select(out=identb[:], in_=onesrc[:, 0:128], pattern=[[-1, 128]], compare_op=ALU.is_equal,
                                   fill=0.0, base=0, channel_multiplier=1), [("pg", 12)], ["identb"])
    LOAD(flag[:], flag_d, "flag")
    LOAD(ln0[:], ln0_d, "ln0")

    def ln_fm(src, dst32, dstbf, gcol, bcol, N, tagk):
        psm, kpm = nextps()
        pse, kpe = nextps()
        rbk = []
        for k in range(8):
            s_ap, s_key = src(k)
            rb_ap, rb_key = HP(16 + k)
            sq_ap, sq_key = HP(24 + k)
            ACT(lambda e, o=rb_ap, i=s_ap: e.activation(out=o[:, 0:N], in_=i, func=AF.Copy), [s_key], [rb_key])
            ACT(lambda e, o=sq_ap, i=s_ap: e.activation(out=o[:, 0:N], in_=i, func=AF.Square), [s_key], [sq_key])
            rbk.append((rb_ap, rb_key, sq_ap, sq_key))

        def mm_stats(e, which):
            last = None
            for k in range(8):
                ap = rbk[k][0] if which == 0 else rbk[k][2]
                dst = psm if which == 0 else pse
                last = e.matmul(dst[:, 0:N], lhsT=onesS[:], rhs=ap[:, 0:N], start=(k == 0), stop=(k == 7))
            return last
        PE(lambda e: mm_stats(e, 0), [r[1] for r in rbk] + ["onesS"], [kpm])
        PE(lambda e: mm_stats(e, 1), [r[3] for r in rbk] + ["onesS"], [kpe])
        mean, kmean = PG(17)
        msq, kmsq = PG(18)
        var, kvar = PG(19)
        rstd_t, krstd = nextps()
        nmr_t, knmr = nextps()
        rstd = rstd_t[:]
        nmr = nmr_t[:]
        ACT(lambda e: e.activation(out=mean[:, 0:N], in_=psm[:, 0:N], func=AF.Copy), [kpm], [kmean])
        ACT(lambda e: e.activation(out=msq[:, 0:N], in_=psm[:, 0:N], func=AF.Square), [kpm], [kmsq])
        DVE(lambda e: e.tensor_tensor(out=var[:, 0:N], in0=pse[:, 0:N], in1=msq[:, 0:N], op=ALU.subtract),
            [kpe, kmsq], [kvar])
        DVE(lambda e: e.tensor_scalar(out=var[:, 0:N], in0=var[:, 0:N], scalar1=0.0, scalar2=LN_EPS,
                                      op0=ALU.max, op1=ALU.add), [kvar], [kvar])
        ACT(lambda e: e.activation(out=var[:, 0:N], in_=var[:, 0:N], func=AF.Sqrt), [kvar], [kvar])
        DVE(lambda e: e.reciprocal(out=rstd[:, 0:N], in_=var[:, 0:N]), [kvar], [krstd])
        DVE(lambda e: e.scalar_tensor_tensor(out=nmr[:, 0:N], in0=mean[:, 0:N], scalar=-1.0, in1=rstd[:, 0:N],
                                             op0=ALU.mult, op1=ALU.mult), [kmean, krstd], [knmr])
        for k in range(8):
            s_ap, s_key = src(k)
            ta, kta = PG(22 + (k % 2))
            DVE(lambda e, o=ta, i=s_ap: e.tensor_tensor(out=o[:, 0:N], in0=rstd[:, 0:N], in1=i, op=ALU.mult),
                [s_key, krstd], [kta])
            DVE(lambda e, o=ta: e.tensor_tensor(out=o[:, 0:N], in0=nmr[:, 0:N], in1=o[:, 0:N], op=ALU.add),
                [kta, knmr], [kta])
            if dst32 is not None:
                d_ap, d_key = dst32(k)
                ACT(lambda e, o=d_ap, i=ta, k=k: e.activation(out=o, in_=i[:, 0:N], func=AF.Identity,
                                                               scale=gcol(k), bias=bcol(k)),
                    [kta, tagk], [d_key])
                if dstbf is not None:
                    b_ap, b_key = dstbf(k)
                    ACT(lambda e, o=b_ap, i=d_ap: e.activation(out=o, in_=i, func=AF.Copy), [d_key], [b_key])
            else:
                b_ap, b_key = dstbf(k)
                ACT(lambda e, o=b_ap, i=ta, k=k: e.activation(out=o, in_=i[:, 0:N], func=AF.Identity,
                                                               scale=gcol(k), bias=bcol(k)),
                    [kta, tagk], [b_key])

    def xs(k, blk):
        return x32[:, k, blk * TB:(blk + 1) * TB], ("x32", k, blk)

    def xbk(k):
        return xb[:, k, :], ("xb", k)

    for blk in range(NB):
        S.dma("sp", lambda e, inc, blk=blk: inc(e.dma_start(
            out=x32[:, :, blk * TB:(blk + 1) * TB],
            in_=xT_d[:, blk * TB:(blk + 1) * TB].rearrange("(k p) t -> p k t", p=128))),
            "xin%d" % blk, writes=[("x32", k, blk) for k in range(8)])
    mst = arena[:, 0:2048].rearrange("p (k t) -> p k t", k=8)
    S.dma("sp", lambda e, inc: inc(e.dma_start(out=mst, in_=memT_d.rearrange("(k p) t -> p k t", p=128))),
          "memin", writes=[("pg", i) for i in range(4)])
    ln_fm(lambda k: (mst[:, k, :], ("pg", k // 2)), None, lambda k: (memT[:, k, :], ("memT", k)),
          lambda k: ln0[:, k, 2:3], lambda k: ln0[:, k, 3:4], 256, "ln0")
    for blk in range(NB):
        ln_fm(lambda k, blk=blk: xs(k, blk), lambda k, blk=blk: xs(k, blk), None,
              lambda k: ln0[:, k, 0:1], lambda k: ln0[:, k, 1:2], TB, "ln0")

    def load_layer_consts(l):
        LOAD(binfm[:], binfm_d[l], "binfm")
        LOAD(lnp[:], lnp_d[l], "lnp")
        LOAD(bpar[:], bpar_d[l], "bpar")
        LOAD(w_r[:], w_r_d[l], "w_r")
        LOAD(b_r[:], b_r_d[l], "b_r")
        LOAD(lnvbc[:, 0, :], lnv_d[l, 0].partition_broadcast(128), "lnvg")
        LOAD(lnvbc[:, 1, :], lnv_d[l, 1].partition_broadcast(128), "lnvb")
        S.dma("pool", lambda e, inc: inc(e.dma_start(out=WmT[:], in_=wsT_d[l])), "WmT", writes=["WmT"])
        POOL(lambda e: e.affine_select(out=WmT[:], in_=WmT[:], pattern=[[0, 8], [1, 128]], compare_op=ALU.is_ge,
                                       fill=0.0, base=0, channel_multiplier=-1), ["WmT"], ["WmT"])
        S.dma("pool", lambda e, inc: inc(e.dma_start(out=waxb[:], in_=wax_d[l].rearrange("a h i j -> i a h j"))),
              "waxb", writes=["waxb"])
        POOL(lambda e: e.memset(brow[:], 0.0), [], ["brow"])
        st, kst0 = PG(22)
        st2, kst1 = PG(23)
        stg = arena[:, 22 * 512:24 * 512]
        browhi32 = arena_bf[:, 42 * 512:44 * 512]
        for j in range(3):
            S.dma("pool", lambda e, inc, j=j: inc(e.dma_start(out=brow[0:1, j * 1024:(j + 1) * 1024],
                                                              in_=brow_d[l, j:j + 1, :])),
                  "browhi%d" % j, reads=["brow"], writes=["brow"])
            S.dma("pool", lambda e, inc, j=j: inc(e.dma_start(out=browhi32[32:33, :], in_=brow_d[l, j:j + 1, :])),
                  "browhi32", writes=[("pg", 21)])
            S.dma("sp", lambda e, inc, j=j: inc(e.dma_start(out=stg[32:33, :], in_=brow_d[l, j:j + 1, :])),
                  "browst", writes=[kst0, kst1])
            DVE(lambda e, j=j: e.tensor_tensor(out=brow[32:33, j * 1024:(j + 1) * 1024], in0=stg[32:33, :],
                                               in1=browhi32[32:33, :], op=ALU.subtract),
                [kst0, kst1, ("pg", 21), "brow"], ["brow"])
        ACT(lambda e: e.activation(out=sptmp[:], in_=bpar[:, :, 7], func=AF.Exp, scale=-1.0), ["bpar"], ["sptmp"])
        ACT(lambda e: e.activation(out=sptmp[:], in_=sptmp[:], func=AF.Ln, bias=1.0), ["sptmp"], ["sptmp"])
        DVE(lambda e: e.tensor_scalar(out=negc[:], in0=sptmp[:], scalar1=-8.0, scalar2=None, op0=ALU.mult),
            ["sptmp"], ["negc"])
        DVE(lambda e: e.tensor_scalar(out=neg2c[:], in0=sptmp[:], scalar1=-16.0, scalar2=None, op0=ALU.mult),
            ["sptmp"], ["neg2c"])
        def kv_compute():
            for ct in range(2):
                wtile, wk = wload(mat_view(w_kv_d, l, 0, 8, ct * 512, 512), 8, 512)
                for dcl in range(4):
                    dc = ct * 4 + dcl
                    ps, kp = nextps()

                    def f(e, ps=ps, wtile=wtile, dcl=dcl):
                        last = None
                        for k in range(8):
                            last = e.matmul(ps[:, 0:256], lhsT=wtile[:, k, dcl * 128:(dcl + 1) * 128], rhs=memT[:, k, :],
                                            start=(k == 0), stop=(k == 7))
                        return last
                    PE(f, [wk] + [("memT", k) for k in range(8)], [kp])
                    ACT(lambda e, ps=ps, dc=dc: e.activation(out=KT[:, dc, :], in_=ps[:, 0:256], func=AF.Copy),
                        [kp], [("KT", dc)])
            for half in range(2):
                wtile, wk = wload(mat_view(w_kv_d, l, 0, 8, 1024 + half * 512, 512), 8, 512)
                for mc in range(2):
                    ps, kp = nextps()

                    def f(e, ps=ps, wtile=wtile, mc=mc):
                        last = None
                        for k in range(8):
                            last = e.matmul(ps[:], lhsT=memT[:, k, mc * 128:(mc + 1) * 128], rhs=wtile[:, k, :],
                                            start=(k == 0), stop=(k == 7))
                        return last
                    PE(f, [wk] + [("memT", k) for k in range(8)], [kp])
                    ACT(lambda e, ps=ps, mc=mc, half=half: e.activation(out=Vt[:, mc, half * 512:(half + 1) * 512],
                                                                        in_=ps[:], func=AF.Copy),
                        [kp], [("Vt", mc, half)])
        return kv_compute

    def cast_xb(blk):
        for k in range(8):
            s_ap, s_key = xs(k, blk)
            ACT(lambda e, k=k, s_ap=s_ap: e.activation(out=xb[:, k, :], in_=s_ap, func=AF.Copy), [s_key], [("xb", k)])

    XBK = [("xb", k) for k in range(8)]

    def mm_fm(ps, wtile, c0, K, rhs_of_k, extra=None):
        def f(e):
            last = None
            for k in range(K):
                last = e.matmul(ps[:], lhsT=wtile[:, k, c0:c0 + 128], rhs=rhs_of_k(k), start=(k == 0),
                                stop=(k == K - 1 and extra is None))
            if extra is not None:
                last = e.matmul(ps[:], lhsT=extra[0], rhs=extra[1], start=False, stop=True)
            return last
        return f

    def b_slots():
        def pgs(lo, n):
            return [("pg", i) for i in range(lo, lo + n)]
        sl = []
        for i in range(4):
            if i == 0:
                xr, kxr = XR[:, :], ["XR"]
            else:
                base = (i - 1) * 2
                xr, kxr = arena[:, base * 512:base * 512 + 516], pgs(base, 2)
            sl.append(dict(xr=xr, kxr=kxr, xc=PG(6 + i), xcb=HP(20 + i), rr=PG(12 + i), ii=PG(16 + i), mm=PG(20 + i),
                           iip=16 + i))
        return sl

    ZP, KZP = PG(24)

    def b_xproj(l, heads, st):
        pss = {}
        for h in heads:
            if h % 4 == 0:
                ncol = min(512, 1280 - h * 128)
                st["x"] = wload(win_view(l, 2048 + h * 128, ncol), 8, ncol)
            xtile, xk = st["x"]
            ps, kp = nextps(pin=True)
            PE(mm_fm(ps, xtile, (h % 4) * 128, 8, lambda k: xb[:, k, :]), [xk] + XBK, [kp])
            pss[h] = (ps, kp)
        return pss

    def b_pass(l, blk):
        sl = b_slots()
        G = len(sl)
        st = {}
        groups = [list(range(h0, min(10, h0 + G))) for h0 in range(0, 10, G)]
        pss_next = b_xproj(l, groups[0], st)
        for gi, heads in enumerate(groups):
            h0 = heads[0]
            pss = pss_next
            for h in heads:
                b = sl[h - h0]
                ps, kp = pss[h]
                xr, kxr = b["xr"], b["kxr"]
                xc, kxc = b["xc"]
                xcb, kxcb = b["xcb"]
                DVE(lambda e, xr=xr, h=h: e.tensor_copy(out=xr[:, 0:3], in_=HALO[:, h, :]), [("halo", h)], kxr)
                ACT(lambda e, xr=xr, ps=ps, h=h: e.activation(out=xr[:, 3:515], in_=ps[:], func=AF.Identity,
                                                               bias=binfm[:, 16 + h:17 + h]), [kp, "binfm"] + kxr, kxr)
                unpin(kp)
                DVE(lambda e, xr=xr, h=h: e.tensor_copy(out=HALO[:, h, :], in_=xr[:, 512:515]), kxr, [("halo", h)])
                DVE(lambda e, xr=xr, xc=xc, h=h: e.tensor_scalar(out=xc, in0=xr[:, 0:512], scalar1=bpar[:, h, 0:1],
                                                                 scalar2=bpar[:, h, 4:5], op0=ALU.mult, op1=ALU.add),
                    kxr + ["bpar"], [kxc])
                for tp in range(1, 4):
                    DVE(lambda e, xr=xr, xc=xc, h=h, tp=tp: e.scalar_tensor_tensor(
                        out=xc, in0=xr[:, tp:tp + 512], scalar=bpar[:, h, tp:tp + 1], in1=xc,
                        op0=ALU.mult, op1=ALU.add), kxr + ["bpar", kxc], [kxc])
                ACT(lambda e, xc=xc, xcb=xcb: e.activation(out=xcb, in_=xc, func=AF.Copy), [kxc], [kxcb])
            if gi + 1 < len(groups):
                pss_next = b_xproj(l, groups[gi + 1], st)
            for h in heads:
                b = sl[h - h0]
                xcb, kxcb = b["xcb"]
                ps_r, kpr = nextps()
                ps_i, kpi = nextps()
                PE(lambda e, ps_r=ps_r, xcb=xcb, h=h: e.matmul(ps_r[:], lhsT=waxb[:, 0, h, :], rhs=xcb, start=True,
                                                               stop=True), ["waxb", kxcb], [kpr])
                PE(lambda e, ps_i=ps_i, xcb=xcb, h=h: e.matmul(ps_i[:], lhsT=waxb[:, 1, h, :], rhs=xcb, start=True,
                                                               stop=True), ["waxb", kxcb], [kpi])
                rr, krr = b["rr"]
                ii, kii = b["ii"]
                ACT(lambda e, ps_r=ps_r, rr=rr, h=h: e.activation(out=rr, in_=ps_r[:], func=AF.Sigmoid,
                                                                  bias=bpar[:, h, 5:6]), [kpr, "bpar"], [krr])
                ACT(lambda e, ps_i=ps_i, ii=ii, h=h: e.activation(out=ii, in_=ps_i[:], func=AF.Sigmoid,
                                                                  bias=bpar[:, h, 6:7]), [kpi, "bpar"], [kii])
            for h in heads:
                b = sl[h - h0]
                rr, krr = b["rr"]
                mm, kmm = b["mm"]
                ACT(lambda e, rr=rr, mm=mm, h=h: e.activation(out=mm, in_=rr, func=AF.Exp, scale=neg2c[:, h:h + 1]),
                    [krr, "neg2c"], [kmm])
                ACT(lambda e, rr=rr, h=h: e.activation(out=rr, in_=rr, func=AF.Exp, scale=negc[:, h:h + 1]),
                    [krr, "negc"], [krr])
                DVE(lambda e, mm=mm: e.tensor_scalar(out=mm, in0=mm, scalar1=-1.0, scalar2=1.0, op0=ALU.mult,
                                                     op1=ALU.add), [kmm], [kmm])
                DVE(lambda e, mm=mm: e.tensor_scalar(out=mm, in0=mm, scalar1=0.0, scalar2=None, op0=ALU.max),
                    [kmm], [kmm])
            for h in heads:
                b = sl[h - h0]
                mm, kmm = b["mm"]
                ACT(lambda e, mm=mm: e.activation(out=mm, in_=mm, func=AF.Sqrt), [kmm], [kmm])
            for h in heads:
                b = sl[h - h0]
                rr, krr = b["rr"]
                ii, kii = b["ii"]
                mm, kmm = b["mm"]
                xc, kxc = b["xc"]
                DVE(lambda e, ii=ii, xc=xc: e.tensor_tensor(out=ii, in0=ii, in1=xc, op=ALU.mult), [kii, kxc], [kii])
                DVE(lambda e, ii=ii, mm=mm: e.tensor_tensor(out=ii, in0=ii, in1=mm, op=ALU.mult), [kii, kmm], [kii])
                DVE(lambda e, xc=xc, rr=rr, ii=ii, h=h: e.tensor_tensor_scan(out=xc, data0=rr, data1=ii,
                                                                            initial=HC[:, h:h + 1], op0=ALU.mult,
                                                                            op1=ALU.add),
                    [krr, kii, ("hc", h), kxc], [kxc])
                DVE(lambda e, xc=xc, h=h: e.tensor_copy(out=HC[:, h:h + 1], in_=xc[:, 511:512]), [kxc], [("hc", h)])
                DVE(lambda e, mm=mm, rr=rr, h=h: e.tensor_tensor_scan(out=mm, data0=rr, data1=ZP,
                                                                      initial=PC[:, h:h + 1], op0=ALU.mult,
                                                                      op1=ALU.add),
                    [krr, KZP, ("pc", h), kmm], [kmm])
                DVE(lambda e, mm=mm, h=h: e.tensor_copy(out=PC[:, h:h + 1], in_=mm[:, 511:512]), [kmm], [("pc", h)])
            for h in heads:
                b = sl[h - h0]
                gg, kgg = b["xcb"]
                if h % 4 == 0:
                    ncol = min(512, 1280 - h * 128)
                    st["g"] = wload(win_view(l, 3328 + h * 128, ncol), 8, ncol)
                gtile, gk = st["g"]
                ps_g, kpg = nextps()
                PE(mm_fm(ps_g, gtile, (h % 4) * 128, 8, lambda k: xb[:, k, :]), [gk] + XBK, [kpg])
                ACT(lambda e, ps_g=ps_g, gg=gg, h=h: e.activation(out=gg, in_=ps_g[:], func=AF.Gelu_apprx_tanh,
                                                                  bias=binfm[:, 26 + h:27 + h]),
                    [kpg, "binfm"], [kgg])
            for h in heads:
                b = sl[h - h0]
                gg, kgg = b["xcb"]
                xc, kxc = b["xc"]
                mm, kmm = b["mm"]
                ii, kii = b["ii"]
                p = b["iip"]
                ol, _ = HP(2 * p)
                pg_, _ = HP(2 * p + 1)
                DVE(lambda e, ol=ol, xc=xc, gg=gg: e.tensor_tensor(out=ol, in0=xc, in1=gg, op=ALU.mult),
                    [kxc, kgg, kii], [kii])
                DVE(lambda e, pg_=pg_, mm=mm, gg=gg: e.tensor_tensor(out=pg_, in0=mm, in1=gg, op=ALU.mult),
                    [kmm, kgg, kii], [kii])
                S.dma("sp", lambda e, inc, p=p, h=h: inc(e.dma_start(
                    out=spill_d[blk, h], in_=arena_bf[:, 2 * p * 512:(2 * p + 2) * 512])),
                    "sp%d" % h, reads=[kii], writes=[("spill", blk, h)])

    LP = [19, 20, 21, 22, 23, 24, 8, 9, 10, 11]

    def b_load(blk, hs):
        for h in hs:
            p = LP[h]
            S.dma("sp", lambda e, inc, p=p, h=h: inc(e.dma_start(
                out=arena_bf[:, 2 * p * 512:(2 * p + 2) * 512], in_=spill_d[blk, h])),
                "ld%d" % h, reads=[("spill", blk, h)], writes=[("pg", p)])

    def b_apply(blk):
        for h in range(10):
            p = LP[h]
            ol, _ = HP(2 * p)
            pg_, _ = HP(2 * p + 1)
            ob, kob = HP(24 + h)
            DVE(lambda e, ol=ol, pg_=pg_, ob=ob, h=h: e.scalar_tensor_tensor(
                out=ob, in0=pg_, scalar=HCI[:, h:h + 1], in1=ol, op0=ALU.mult, op1=ALU.add),
                [("pg", p), "hci"], [kob])

    def project_gate(l, o_tiles, pd, nK, gate_col0, first, last):
        for ct in range(2):
            if nK == 8:
                ptiles = [wload(mat_view(pd, l, 0, 8, ct * 512, 512), 8, 512)]
                kmap = [(0, k) for k in range(8)]
            else:
                ptiles = [wload(mat_view(pd, l, 0, 5, ct * 512, 512), 5, 512),
                          wload(mat_view(pd, l, 640, 5, ct * 512, 512), 5, 512)]
                kmap = [(0, k) for k in range(5)] + [(1, k) for k in range(5)]
            gtile, gk = wload(win_view(l, gate_col0 + ct * 512, 512), 8, 512)
            for dcl in range(4):
                dc = ct * 4 + dcl
                ps_g, kpg = nextps()
                PE(mm_fm(ps_g, gtile, dcl * 128, 8, lambda k: xb[:, k, :]), [gk] + XBK, [kpg])
                ps_p, kpp = nextps()

                def f(e, ps_p=ps_p, dcl=dcl, ptiles=ptiles):
                    lasti = None
                    for idx, (ti, kk) in enumerate(kmap):
                        lasti = e.matmul(ps_p[:], lhsT=ptiles[ti][0][:, kk, dcl * 128:(dcl + 1) * 128],
                                         rhs=o_tiles[idx][0], start=(idx == 0), stop=(idx == nK - 1))
                    return lasti
                PE(f, [t[1] for t in ptiles] + [t[1] for t in o_tiles], [kpp])
                gs, kgs = PG(17 + (dc % 2))
                ecol = gate_col0 // 128 + dc
                ACT(lambda e, ps_g=ps_g, gs=gs, ecol=ecol: e.activation(out=gs, in_=ps_g[:], func=AF.Sigmoid,
                                                                        bias=binfm[:, ecol:ecol + 1]),
                    [kpg, "binfm"], [kgs])
                y, ky = PG(dc)
                if first:
                    DVE(lambda e, y=y, ps_p=ps_p, gs=gs: e.tensor_tensor(out=y, in0=ps_p[:], in1=gs, op=ALU.mult),
                        [kpp, kgs], [ky])
                else:
                    DVE(lambda e, ps_p=ps_p, gs=gs: e.tensor_tensor(out=gs, in0=ps_p[:], in1=gs, op=ALU.mult),
                        [kpp, kgs], [kgs])
                    if not last:
                        DVE(lambda e, y=y, gs=gs: e.tensor_tensor(out=y, in0=y, in1=gs, op=ALU.add),
                            [ky, kgs], [ky])
                    else:
                        yb, kyb = HP(16 + dc)
                        DVE(lambda e, y=y, gs=gs, yb=yb: e.tensor_tensor(out=yb, in0=y, in1=gs, op=ALU.add),
                            [ky, kgs], [kyb])

    for l in range(n_layers):
        kv_compute = load_layer_consts(l)
        hck = [("hc", h) for h in range(10)]
        hak = [("halo", h) for h in range(10)]
        pck = [("pc", h) for h in range(10)]
        cast_xb(NB - 1)
        st0 = {}
        for h0 in range(0, 10, 4):
            heads = list(range(h0, min(10, h0 + 4)))
            pss = b_xproj(l, heads, st0)
            for h in heads:
                ps, kp = pss[h]
                ACT(lambda e, ps=ps, h=h: e.activation(out=EX[:, 10 + 3 * h:13 + 3 * h], in_=ps[:, 509:512],
                                                        func=AF.Identity, bias=binfm[:, 16 + h:17 + h]),
                    [kp, "binfm"], [("exh", h)])
                unpin(kp)
        S.dma("sp", lambda e, inc: inc(e.dma_start(out=cc_in1, in_=EX[:, 10:40])), "ccin1",
              reads=[("exh", h) for h in range(10)], writes=["ccin1"])
        POOL(lambda e: e.collective_compute("AllGather", ALU.bypass,
                                            replica_groups=[[0, 1], [2, 3], [4, 5], [6, 7]],
                                            ins=[cc_in1], outs=[cc_out1]), ["ccin1"], ["ccout1"])
        S.dma("sp", lambda e, inc: inc(e.dma_start(out=RX[:, 10:40], in_=cc_out1[0:128, :])), "rx1", reads=["ccout1"],
              writes=["RX1"])
        kv_compute()
        DVE(lambda e: e.tensor_scalar(out=HALO[:].rearrange("p h t -> p (h t)"), in0=RX[:, 10:40],
                                      scalar1=flag[:, 0:1], scalar2=None, op0=ALU.mult), ["RX1", "flag"], hak)
        DVE(lambda e: e.memset(HC[:], 0.0), [], hck)
        DVE(lambda e: e.memset(PC[:], 1.0), [], pck)
        DVE(lambda e: e.memset(ZP, 0.0), [], [KZP])
        for blk in range(NB):
            cast_xb(blk)
            b_pass(l, blk)
        DVE(lambda e: e.tensor_copy(out=EX[:, 0:10], in_=HC[:]), hck, ["exs"])
        S.dma("sp", lambda e, inc: inc(e.dma_start(out=cc_in2, in_=EX[:, 0:10])), "ccin2", reads=["exs"],
              writes=["ccin2"])
        POOL(lambda e: e.collective_compute("AllGather", ALU.bypass,
                                            replica_groups=[[0, 1], [2, 3], [4, 5], [6, 7]],
                                            ins=[cc_in2], outs=[cc_out2]), ["ccin2"], ["ccout2"])
        S.dma("sp", lambda e, inc: inc(e.dma_start(out=RX[:, 0:10], in_=cc_out2[0:128, :])), "rx2", reads=["ccout2"],
              writes=["RX2"])
        DVE(lambda e: e.tensor_scalar(out=HCI[:], in0=RX[:, 0:10], scalar1=flag[:, 0:1], scalar2=None, op0=ALU.mult),
            ["RX2", "flag"], ["hci"])

        branches = dbg[1] if (dbg and dbg[0] == "y") else "ABC"
        ydbg = bool(dbg and dbg[0] == "y")
        for blk in range(NB):
            cast_xb(blk)
            if dbg and dbg[0] == "x0":
                S.dma("sp", lambda e, inc, blk=blk: inc(e.dma_start(
                    out=out_d[:, blk * TB:(blk + 1) * TB].rearrange("(k p) t -> p k t", p=128),
                    in_=x32[:, :, blk * TB:(blk + 1) * TB])), "out%d" % blk,
                    reads=[("x32", k, blk) for k in range(8)], writes=[("out", blk)])
                continue
            if "A" in branches:
                for ct in range(2):
                    utile, uk = wload(win_view(l, ct * 512, 512), 8, 512)
                    for dcl in range(4):
                        dc = ct * 4 + dcl
                        ps, kp = nextps()
                        PE(mm_fm(ps, utile, dcl * 128, 8, lambda k: xb[:, k, :]), [uk] + XBK, [kp])
                        u_ap, u_key = HP(16 + dc)
                        ACT(lambda e, ps=ps, u_ap=u_ap, dc=dc: e.activation(out=u_ap, in_=ps[:], func=AF.Gelu_apprx_tanh,
                                                                            bias=binfm[:, dc:dc + 1]),
                            [kp, "binfm"], [u_key])
                vt0, vk0 = wload(win_view(l, 1024, 512), 8, 512)
                vt1, vk1 = wload(win_view(l, 1536, 512), 8, 512)
                def a_vmm(tc_):
                    pss = []
                    for half, (vt, vk) in enumerate(((vt0, vk0), (vt1, vk1))):
                        ps, kp = nextps()

                        def f(e, ps=ps, vt=vt, half=half, tc_=tc_):
                            for k in range(8):
                                e.matmul(ps[:], lhsT=xb[:, k, tc_ * 128:(tc_ + 1) * 128], rhs=vt[:, k, :],
                                         start=(k == 0), stop=False)
                            return e.matmul(ps[:], lhsT=ones33[:, 0:128], rhs=brow[:, half * 512:(half + 1) * 512],
                                            start=False, stop=True)
                        PE(f, [vk, "brow", "ones33"] + XBK, [kp])
                        pss.append((ps, kp))
                    return pss

                def a_chain(tc_, pss):
                    for half, (ps, kp) in enumerate(pss):
                        ACT(lambda e, ps=ps, half=half: e.activation(out=gv[:, half * 512:(half + 1) * 512], in_=ps[:],
                                                                      func=AF.Gelu_apprx_tanh,
                                                                      accum_out=stat[:, half:half + 1]),
                            [kp], [("gv", half), ("stat", half)])
                    for half in range(2):
                        ACT(lambda e, half=half: e.activation(out=PG(23 + half)[0],
                                                              in_=gv[:, half * 512:(half + 1) * 512], func=AF.Square,
                                                              accum_out=stat[:, 2 + half:3 + half]),
                            [("gv", half)], [PG(23 + half)[1], ("stat", 2 + half)])
                    stk = [("stat", i) for i in range(4)]
                    DVE(lambda e: e.tensor_tensor(out=stat[:, 4:5], in0=stat[:, 0:1], in1=stat[:, 1:2], op=ALU.add),
                        stk, ["st4"])
                    DVE(lambda e: e.tensor_tensor(out=stat[:, 5:6], in0=stat[:, 2:3], in1=stat[:, 3:4], op=ALU.add),
                        stk, ["st5"])
                    DVE(lambda e: e.tensor_scalar(out=stat[:, 4:6], in0=stat[:, 4:6], scalar1=1.0 / 1024.0, scalar2=None,
                                                  op0=ALU.mult), ["st4", "st5"], ["st4", "st5"])
                    DVE(lambda e: e.tensor_tensor(out=stat[:, 6:7], in0=stat[:, 4:5], in1=stat[:, 4:5], op=ALU.mult),
                        ["st4"], ["st6"])
                    DVE(lambda e: e.tensor_tensor(out=stat[:, 6:7], in0=stat[:, 5:6], in1=stat[:, 6:7], op=ALU.subtract),
                        ["st5", "st6"], ["st6"])
                    DVE(lambda e: e.tensor_scalar(out=stat[:, 6:7], in0=stat[:, 6:7], scalar1=0.0, scalar2=LN_EPS,
                                                  op0=ALU.max, op1=ALU.add), ["st6"], ["st6"])
                    ACT(lambda e: e.activation(out=stat[:, 6:7], in_=stat[:, 6:7], func=AF.Sqrt), ["st6"], ["st6"])
                    DVE(lambda e: e.reciprocal(out=stat[:, 7:8], in_=stat[:, 6:7]), ["st6"], ["st7"])
                    DVE(lambda e: e.scalar_tensor_tensor(out=stat[:, 8:9], in0=stat[:, 4:5], scalar=-1.0,
                                                         in1=stat[:, 7:8], op0=ALU.mult, op1=ALU.mult),
                        ["st4", "st7"], ["st8"])
                    for half in range(2):
                        sl = slice(half * 512, (half + 1) * 512)
                        DVE(lambda e, sl=sl: e.tensor_scalar(out=gv[:, sl], in0=gv[:, sl], scalar1=stat[:, 7:8],
                                                             scalar2=stat[:, 8:9], op0=ALU.mult, op1=ALU.add),
                            [("gv", half), "st7", "st8"], [("gv", half)])
                        DVE(lambda e, sl=sl: e.tensor_tensor(out=gv[:, sl], in0=gv[:, sl], in1=lnvbc[:, 0, sl],
                                                             op=ALU.mult), [("gv", half), "lnvg"], [("gv", half)])
                        DVE(lambda e, sl=sl: e.tensor_tensor(out=vn[:, sl], in0=gv[:, sl], in1=lnvbc[:, 1, sl],
                                                             op=ALU.add), [("gv", half), "lnvb"], [("vn", half)])

                def a_mix(tc_):
                    for gh in range(2):
                        ps, kp = nextps()

                        def f(e, ps=ps, gh=gh):
                            last = None
                            for gl in range(4):
                                g = gh * 4 + gl
                                e.matmul(ps[:, gl * 128:(gl + 1) * 128], lhsT=vn[:, g * 128:(g + 1) * 128],
                                         rhs=WmT[:, g, :], start=True, stop=False)
                                last = e.matmul(ps[:, gl * 128:(gl + 1) * 128], lhsT=ones33[:, 0:128],
                                                rhs=brow[:, 1024 + g * 128:1024 + (g + 1) * 128], start=False, stop=True)
                            return last
                        PE(f, [("vn", gh), "WmT", "brow", "ones33"], [kp])
                        for gl in range(4):
                            g = gh * 4 + gl
                            u_ap, u_key = HP(16 + g)
                            DVE(lambda e, ps=ps, gl=gl, u_ap=u_ap, tc_=tc_: e.tensor_tensor(
                                out=u_ap[:, tc_ * 128:(tc_ + 1) * 128], in0=ps[:, gl * 128:(gl + 1) * 128],
                                in1=u_ap[:, tc_ * 128:(tc_ + 1) * 128], op=ALU.mult), [kp, u_key], [u_key])

                pss_cur = a_vmm(0)
                a_chain(0, pss_cur)
                for tc_ in range(4):
                    if tc_ + 1 < 4:
                        pss_nxt = a_vmm(tc_ + 1)
                    a_mix(tc_)
                    if tc_ + 1 < 4:
                        a_chain(tc_ + 1, pss_nxt)
                if "B" in branches:
                    b_load(blk, range(0, 6))
                project_gate(l, [HP(16 + k) for k in range(8)], p_a_d, 8, 5632, True, False)

            if "B" in branches:
                b_load(blk, range(6, 10))
                b_apply(blk)
                project_gate(l, [HP(24 + h) for h in range(10)], p_b_d, 10, 5632 + 1024, branches[0] == 'B', False)

            if "C" in branches:
                for ct in range(2):
                    qtile, qk = wload(win_view(l, 4608 + ct * 512, 512), 8, 512)
                    for dcl in range(4):
                        dc = ct * 4 + dcl
                        ps, kp = nextps()
                        PE(mm_fm(ps, qtile, dcl * 128, 8, lambda k: xb[:, k, :]), [qk] + XBK, [kp])
                        q_ap, q_key = HP(34 + dc)
                        ACT(lambda e, ps=ps, q_ap=q_ap, dc=dc: e.activation(out=q_ap, in_=ps[:], func=AF.Identity,
                                                                            bias=binfm[:, 36 + dc:37 + dc]),
                            [kp, "binfm"], [q_key])
                for hd in range(4):
                    ekeys = []
                    for mc in range(2):
                        ps, kp = nextps()

                        def f(e, ps=ps, hd=hd, mc=mc):
                            e.matmul(ps[:], lhsT=KT[:, hd * 2, mc * 128:(mc + 1) * 128], rhs=HP(34 + hd * 2)[0],
                                     start=True, stop=False)
                            return e.matmul(ps[:], lhsT=KT[:, hd * 2 + 1, mc * 128:(mc + 1) * 128],
                                            rhs=HP(34 + hd * 2 + 1)[0], start=False, stop=True)
                        PE(f, [("KT", hd * 2), ("KT", hd * 2 + 1), HP(34 + hd * 2)[1], HP(34 + hd * 2 + 1)[1]], [kp])
                        ei = (hd % 2) * 2 + mc
                        ACT(lambda e, ps=ps, ei=ei: e.activation(out=HP(24 + ei)[0], in_=ps[:], func=AF.Exp, scale=1.0 / 16.0),
                            [kp], [HP(24 + ei)[1]])
                        ekeys.append(HP(24 + ei)[1])
                    e0 = (hd % 2) * 2
                    ps_d, kpd = nextps()
                    PE(lambda e, ps_d=ps_d, e0=e0: (e.matmul(ps_d[:], lhsT=onesB[:], rhs=HP(24 + e0)[0], start=True, stop=False),
                                                     e.matmul(ps_d[:], lhsT=onesB[:], rhs=HP(24 + e0 + 1)[0], start=False,
                                                              stop=True))[1], ekeys + ["onesB"], [kpd])
                    rden, krd = PG(14 + (hd % 2))
                    DVE(lambda e, ps_d=ps_d, rden=rden: e.reciprocal(out=rden, in_=ps_d[:]), [kpd], [krd])
                    for dl in range(2):
                        ps_o, kpo = nextps()
                        c0 = hd * 256 + dl * 128
                        PE(lambda e, ps_o=ps_o, c0=c0, e0=e0: (
                            e.matmul(ps_o[:], lhsT=Vt[:, 0, c0:c0 + 128], rhs=HP(24 + e0)[0], start=True, stop=False),
                            e.matmul(ps_o[:], lhsT=Vt[:, 1, c0:c0 + 128], rhs=HP(24 + e0 + 1)[0], start=False, stop=True))[1],
                           ekeys + [("Vt", 0, c0 // 512), ("Vt", 1, c0 // 512)], [kpo])
                        oc, koc = HP(42 + hd * 2 + dl)
                        DVE(lambda e, ps_o=ps_o, oc=oc, rden=rden: e.tensor_tensor(out=oc, in0=ps_o[:], in1=rden,
                                                                                    op=ALU.mult), [kpo, krd], [koc])
                project_gate(l, [HP(42 + k) for k in range(8)], p_c_d, 8, 5632 + 2048, branches[0] == 'C', not ydbg)

            if ydbg:
                S.dma("sp", lambda e, inc, blk=blk: inc(e.dma_start(
                    out=out_d[:, blk * TB:(blk + 1) * TB].rearrange("(k p) t -> p k t", p=128),
                    in_=arena[:, 0:4096].rearrange("p (k t) -> p k t", k=8))), "out%d" % blk,
                    reads=[("pg", k) for k in range(8)], writes=[("out", blk)])
                continue
            for ct in range(2):
                otile, ok_ = wload(mat_view(w_o_d, l, 0, 8, ct * 512, 512), 8, 512)
                for dcl in range(4):
                    dc = ct * 4 + dcl
                    ps, kp = nextps()
                    PE(mm_fm(ps, otile, dcl * 128, 8, lambda k: HP(16 + k)[0],
                             extra=(brow[:, 2048 + dc * 128:2048 + (dc + 1) * 128], ones33[:, :])),
                       [ok_, "brow", "ones33"] + [HP(16 + k)[1] for k in range(8)], [kp])
                    x_ap, x_key = xs(dc, blk)
                    DVE(lambda e, ps=ps, x_ap=x_ap: e.scalar_tensor_tensor(out=x_ap, in0=x_ap, scalar=ALPHA,
                                                                            in1=ps[:], op0=ALU.mult, op1=ALU.add),
                        [kp, x_key], [x_key])
            ln_fm(lambda k, blk=blk: xs(k, blk), lambda k, blk=blk: xs(k, blk), xbk,
                  lambda k: lnp[:, k, 0:1], lambda k: lnp[:, k, 1:2], TB, "lnp")

            if dbg and dbg[0] == "x1":
                S.dma("sp", lambda e, inc, blk=blk: inc(e.dma_start(
                    out=out_d[:, blk * TB:(blk + 1) * TB].rearrange("(k p) t -> p k t", p=128),
                    in_=x32[:, :, blk * TB:(blk + 1) * TB])), "out%d" % blk,
                    reads=[("x32", k, blk) for k in range(8)], writes=[("out", blk)])
                continue
            L = lambda c, a, b: rt[:, c, a:b]
            for c in range(4):
                ps, kp = nextps()

                def f(e, ps=ps, c=c, blk=blk):
                    for k in range(8):
                        e.matmul(ps[:, 0:20], lhsT=x32[:, k, blk * TB + c * 128:blk * TB + (c + 1) * 128],
                                 rhs=w_r[:, k, :], start=(k == 0), stop=False)
                    return e.matmul(ps[:, 0:20], lhsT=ones32r[:, :], rhs=b_r[:, :], start=False, stop=True)
                PE(f, [("x32", k, blk) for k in range(8)] + ["w_r", "b_r", "ones32r"], [kp])
                rk = ("rt", c)
                DVE(lambda e, ps=ps, c=c: e.tensor_copy(out=L(c, 0, 20), in_=ps[:, 0:20]), [kp], [rk])
                DVE(lambda e, c=c: e.tensor_reduce(out=L(c, 20, 21), in_=L(c, 0, 4), axis=AX.X, op=ALU.max), [rk], [rk])
                DVE(lambda e, c=c: e.tensor_scalar(out=L(c, 21, 22), in0=L(c, 20, 21), scalar1=-1.0, scalar2=None,
                                                   op0=ALU.mult), [rk], [rk])
                DVE(lambda e, c=c: e.tensor_scalar(out=L(c, 24, 28), in0=L(c, 0, 4), scalar1=L(c, 20, 21),
                                                   scalar2=None, op0=ALU.is_ge), [rk], [rk])
                DVE(lambda e, c=c: e.tensor_scalar(out=L(c, 24, 28), in0=L(c, 24, 28), scalar1=-1.0, scalar2=-NEG,
                                                   op0=ALU.add, op1=ALU.mult), [rk], [rk])
                for g in range(4):
                    DVE(lambda e, c=c, g=g: e.tensor_scalar(out=L(c, 28 + 4 * g, 32 + 4 * g),
                                                            in0=L(c, 4 + 4 * g, 8 + 4 * g),
                                                            scalar1=L(c, 24 + g, 25 + g), scalar2=None, op0=ALU.add),
                        [rk], [rk])
                DVE(lambda e, c=c: e.tensor_reduce(out=L(c, 44, 45), in_=L(c, 28, 44), axis=AX.X, op=ALU.max), [rk], [rk])
                DVE(lambda e, c=c: e.tensor_scalar(out=L(c, 45, 46), in0=L(c, 44, 45), scalar1=-1.0, scalar2=None,
                                                   op0=ALU.mult), [rk], [rk])
                DVE(lambda e, c=c: e.tensor_scalar(out=L(c, 48, 64), in0=L(c, 28, 44), scalar1=L(c, 44, 45),
                                                   scalar2=NEG, op0=ALU.is_ge, op1=ALU.mult), [rk], [rk])
                DVE(lambda e, c=c: e.tensor_tensor(out=L(c, 48, 64), in0=L(c, 48, 64), in1=L(c, 28, 44), op=ALU.add),
                    [rk], [rk])
                DVE(lambda e, c=c: e.tensor_reduce(out=L(c, 46, 47), in_=L(c, 48, 64), axis=AX.X, op=ALU.max), [rk], [rk])
                DVE(lambda e, c=c: e.tensor_scalar(out=L(c, 80, 96), in0=L(c, 28, 44), scalar1=L(c, 46, 47),
                                                   scalar2=None, op0=ALU.is_ge), [rk], [rk])
                DVE(lambda e, c=c: e.tensor_tensor(out=L(c, 47, 48), in0=L(c, 46, 47), in1=L(c, 44, 45),
                                                   op=ALU.subtract), [rk], [rk])
            for c in range(4):
                rk = ("rt", c)
                ACT(lambda e, c=c: e.activation(out=L(c, 64, 68), in_=L(c, 0, 4), func=AF.Exp, bias=L(c, 21, 22),
                                                accum_out=L(c, 22, 23)), [rk], [rk])
                ACT(lambda e, c=c: e.activation(out=L(c, 64, 80), in_=L(c, 28, 44), func=AF.Exp, bias=L(c, 45, 46)),
                    [rk], [rk])
                ACT(lambda e, c=c: e.activation(out=L(c, 47, 48), in_=L(c, 47, 48), func=AF.Exp), [rk], [rk])
            for c in range(4):
                rk = ("rt", c)
                DVE(lambda e, c=c: e.tensor_scalar(out=L(c, 47, 48), in0=L(c, 47, 48), scalar1=1.0, scalar2=None,
                                                   op0=ALU.add), [rk], [rk])
                DVE(lambda e, c=c: e.tensor_tensor(out=L(c, 47, 48), in0=L(c, 47, 48), in1=L(c, 22, 23), op=ALU.mult),
                    [rk], [rk])
                DVE(lambda e, c=c: e.reciprocal(out=L(c, 23, 24), in_=L(c, 47, 48)), [rk], [rk])
                DVE(lambda e, c=c: e.scalar_tensor_tensor(out=L(c, 80, 96), in0=L(c, 64, 80), scalar=L(c, 23, 24),
                                                          in1=L(c, 80, 96), op0=ALU.mult, op1=ALU.mult), [rk], [rk])
                DVE(lambda e, c=c: e.tensor_copy(out=G2[:, c, 0:16], in_=L(c, 80, 96)), [rk], [("G2", c)])
                DVE(lambda e, c=c: e.tensor_tensor(out=G2[:, c, 16:32], in0=L(c, 80, 96), in1=G2[:, c, 0:16],
                                                   op=ALU.subtract), [rk, ("G2", c)], [("G2", c)])
            gT2, kgT2 = HP(42)

            def emit_transpose():
                psT, kpT = nextps()
                psTb = psT[:].bitcast(BF16)
                PE(lambda e: [e.transpose(out=psTb[0:32, c * 128:(c + 1) * 128], in_=G2[:, c, :], identity=identb[:])
                              for c in range(4)][-1], [("G2", c) for c in range(4)] + ["identb"], [kpT])
                ACT(lambda e: e.activation(out=gT2[0:32, :], in_=psTb[0:32, 0:512], func=AF.Copy), [kpT], [kgT2])

            def emit_gb_h(ex, tl):
                ps_gb, kgb = nextps()
                PE(lambda e: e.matmul(ps_gb[:], lhsT=SEL2[:, ex, :], rhs=gT2[0:32, :], start=True, stop=True),
                   ["SEL2", kgT2], [kgb])
                for j, (t_ap, t_key) in enumerate(tl):
                    h_ap, h_key = HP(ex * 2 + j)
                    DVE(lambda e, t_ap=t_ap, h_ap=h_ap: e.tensor_tensor(out=h_ap, in0=ps_gb[:], in1=t_ap, op=ALU.mult),
                        [kgb, t_key], [h_key])

            pending = []
            for ex in range(16):
                wu, wuk = wload(w_up_d[l, ex].rearrange("(k p) c -> p k c", p=128), 8, 512)
                tl = []
                for j in range(2):
                    ps_g, kpg = nextps()
                    ps_u, kpu = nextps()
                    PE(mm_fm(ps_g, wu, j * 128, 8, lambda k: xb[:, k, :]), [wuk] + XBK, [kpg])
                    PE(mm_fm(ps_u, wu, 256 + j * 128, 8, lambda k: xb[:, k, :]), [wuk] + XBK, [kpu])
                    s_ap, s_key = PG(17 + j)
                    t_ap, t_key = PG((19 if ex % 2 == 0 else 22) + j)
                    ACT(lambda e, ps_g=ps_g, s_ap=s_ap: e.activation(out=s_ap, in_=ps_g[:], func=AF.Silu),
                        [kpg], [s_key])
                    DVE(lambda e, ps_u=ps_u, s_ap=s_ap, t_ap=t_ap: e.tensor_tensor(out=t_ap, in0=ps_u[:], in1=s_ap,
                                                                                    op=ALU.mult),
                        [kpu, s_key], [t_key])
                    tl.append((t_ap, t_key))
                pending.append((ex, tl))
                if ex == 1:
                    emit_transpose()
                if ex >= 1:
                    for p in pending:
                        emit_gb_h(*p)
                    pending = []
            for dc in range(8):
                s = w_ctr[0] % NSLOT
                w_ctr[0] += 1
                wd = wt[s][:, :].rearrange("p (i d) -> p i d", i=32)
                wdk = ("w", s)
                S.dma("pool", lambda e, inc, s=s, dc=dc, l=l: inc(e.dma_start(out=wt[s][:, :], in_=w_dn_d[l, dc])),
                      "w%d" % s, writes=[wdk])
                ps, kp = nextps()

                def f(e, ps=ps, wd=wd):
                    last = None
                    for i in range(32):
                        last = e.matmul(ps[:], lhsT=wd[:, i, :], rhs=HP(i)[0], start=(i == 0), stop=(i == 31))
                    return last
                PE(f, [wdk] + [("pg", i) for i in range(16)], [kp])
                x_ap, x_key = xs(dc, blk)
                DVE(lambda e, ps=ps, x_ap=x_ap: e.scalar_tensor_tensor(out=x_ap, in0=x_ap, scalar=ALPHA, in1=ps[:],
                                                                        op0=ALU.mult, op1=ALU.add),
                    [kp, x_key], [x_key])
            ln_fm(lambda k, blk=blk: xs(k, blk), lambda k, blk=blk: xs(k, blk), None,
                  lambda k: lnp[:, k, 2:3], lambda k: lnp[:, k, 3:4], TB, "lnp")
            if l == n_layers - 1:
                S.dma("sp", lambda e, inc, blk=blk: inc(e.dma_start(
                    out=out_d[:, blk * TB:(blk + 1) * TB].rearrange("(k p) t -> p k t", p=128),
                    in_=x32[:, :, blk * TB:(blk + 1) * TB])), "out%d" % blk,
                    reads=[("x32", k, blk) for k in range(8)], writes=[("out", blk)])
    S.wait_all("sp", [("out", blk) for blk in range(NB)])
    S.emit()
    S.close()
    return nc


def _fm(v):
    return np.ascontiguousarray(v.reshape(8, 128).T)


def prep_inputs(inp):
    f = lambda a: np.ascontiguousarray(np.asarray(a, dtype=np.float32))
    D = DEPTH
    shared = {}
    shared["ln0"] = f(np.stack([_fm(inp["ln_in_g"]), _fm(inp["ln_in_b"]), _fm(inp["ln_mem_g"]), _fm(inp["ln_mem_b"])], axis=-1))
    shared["w_in"] = f(inp["w_in"])
    shared["bin_fm"] = f(np.stack([np.asarray(inp["b_in"][l]).reshape(68, 128).T for l in range(D)]))
    shared["brow"] = f(np.stack([np.stack([np.asarray(inp["b_in"][l][1024:2048]), np.asarray(inp["b_s"][l]).reshape(1024),
                                           np.asarray(inp["b_o"][l])]) for l in range(D)]))
    shared["lnv"] = f(np.stack([np.stack([inp["ln_v_g"][l], inp["ln_v_b"][l]]) for l in range(D)]))
    shared["wsT"] = f(np.stack([np.asarray(inp["w_s"][l]).transpose(2, 0, 1) for l in range(D)]))
    bp = []
    for l in range(D):
        cw = np.asarray(inp["conv_w"][l]).reshape(4, 10, 128).transpose(2, 1, 0)
        others = [np.asarray(inp[n][l]).reshape(10, 128).T[:, :, None] for n in ("conv_b", "b_a", "b_x", "lam")]
        bp.append(np.concatenate([cw] + others, axis=-1))
    shared["bpar"] = f(np.stack(bp))
    shared["wax"] = f(np.stack([np.stack([inp["w_a"][l], inp["w_x"][l]]) for l in range(D)]))
    shared["w_kv"] = f(inp["w_kv"])
    shared["p_a"] = f(inp["p_a"])
    shared["p_b"] = f(inp["p_b"])
    shared["p_c"] = f(inp["p_c"])
    shared["w_o"] = f(inp["w_o"])
    shared["lnp"] = f(np.stack([np.stack([_fm(inp["ln1_g"][l]), _fm(inp["ln1_b"][l]), _fm(inp["ln2_g"][l]),
                                          _fm(inp["ln2_b"][l])], axis=-1) for l in range(D)]))
    wr = []
    for l in range(D):
        w = np.concatenate([np.asarray(inp["w_rg"][l]), np.asarray(inp["w_re"][l])], axis=1)
        wr.append(w.reshape(8, 128, 20).transpose(1, 0, 2))
    shared["w_r"] = f(np.stack(wr))
    shared["b_r"] = f(np.stack([np.concatenate([np.asarray(inp["b_rg"][l]), np.asarray(inp["b_re"][l])])[None, :]
                                for l in range(D)]))
    shared["w_up"] = f(inp["w_up"])
    wd = []
    for l in range(D):
        w = np.asarray(inp["w_down"][l]).reshape(16, 2, 128, 8, 128)
        wd.append(w.transpose(3, 2, 0, 1, 4).reshape(8, 128, 32 * 128))
    shared["w_dn"] = f(np.stack(wd))
    x = np.asarray(inp["x"], dtype=np.float32)
    mem = np.asarray(inp["mem"], dtype=np.float32)
    in_maps = []
    for c in range(NCORES):
        b, hh = c // 2, c % 2
        m = dict(shared)
        m["xT"] = np.ascontiguousarray(x[b, hh * NT:(hh + 1) * NT, :].T)
        m["memT"] = np.ascontiguousarray(mem[b].T)
        m["flag"] = np.full((128, 1), float(hh), np.float32)
        in_maps.append(m)
    return in_maps


_NC_CACHE = {}


def kernel(_dbg=None, _nl=DEPTH, **inputs):
    in_maps = prep_inputs(inputs)
    if _dbg is not None or _nl != DEPTH:
        _NC_CACHE["nc"] = build_program(_nl, _dbg)
    if "nc" not in _NC_CACHE:
        _NC_CACHE["nc"] = build_program()
    nc = _NC_CACHE["nc"]
    res = run_bass_kernel_spmd(nc, in_maps, core_ids=list(range(NCORES)))
    out = np.empty((4, 4096, 1024), np.float32)
    for c in range(NCORES):
        b, hh = c // 2, c % 2
        out[b, hh * NT:(hh + 1) * NT, :] = res.results[c]["out"].T
    return out
```
